# Optimizing a Trainium2 kernel written in Bass

```python
import math
import jax, jax.numpy as jnp
from jax import lax
import numpy as np

D_MODEL = 2048
BATCH = 8
SEQ = 2048
DEPTH = 2

HEAD_DIM = 128
A_HEADS = 12
A_WIDTH = A_HEADS * HEAD_DIM
DILATED_CONFIGS = ((128, 1), (512, 4), (2048, 16))
POOL_WINDOWS = (2, 4, 8, 16)
POOL_GROUP = 128
B_WIDTH = len(POOL_WINDOWS) * POOL_GROUP
EVEN_IN_WIDTH = 3 * A_WIDTH + B_WIDTH
MIX_WIDTH = A_WIDTH + B_WIDTH
HGRN_HEADS = 16
HGRN_HEAD_DIM = 128
HGRN_WIDTH = HGRN_HEADS * HGRN_HEAD_DIM
HGRN_CHUNK = 64
D_FF = 5632
N_EXPERTS = 8
TOP_K = 2
D_EXPERT = 7168
MOE_BLOCK_ROWS = 512
N_EVEN = (DEPTH + 1) // 2
N_ODD = DEPTH // 2
EPS = 1e-6

kernel_name = "hybrid_dilated_pool_hgrn2_moe"


def rms_norm(x, g):
    xf = x.astype(jnp.float32)
    y = xf * lax.rsqrt(jnp.mean(xf * xf, axis=-1, keepdims=True) + EPS)
    return (y * g.astype(jnp.float32)).astype(x.dtype)


def dilated_branch(q, k, v, window, dil):
    B, S, H, hd = q.shape
    steps = window // dil
    L = S // dil
    nb = -(-L // steps)
    Lp = nb * steps

    def to_res(t):
        t = t.reshape(B, L, dil, H, hd).transpose(0, 2, 3, 1, 4)
        t = jnp.pad(t, ((0, 0), (0, 0), (0, 0), (0, Lp - L), (0, 0)))
        return t.reshape(B, dil, H, nb, steps, hd)

    def with_prev(t):
        prev = jnp.pad(t, ((0, 0), (0, 0), (0, 0), (1, 0), (0, 0), (0, 0)))[:, :, :, :-1]
        return jnp.concatenate([prev, t], axis=-2)

    qb = to_res(q)
    kk = with_prev(to_res(k))
    vv = with_prev(to_res(v))
    s = jnp.einsum('brhnqc,brhnkc->brhnqk', qb, kk).astype(jnp.float32) / math.sqrt(hd)
    n_idx = jnp.arange(nb)[:, None, None]
    i = jnp.arange(steps)[None, :, None]
    j = jnp.arange(2 * steps)[None, None, :]
    mask = (j >= i) & (j <= i + steps) & ((n_idx > 0) | (j >= steps))
    s = jnp.where(mask, s, -jnp.inf)
    lse = jax.nn.logsumexp(s, axis=-1)
    p = jnp.exp(s - lse[..., None])
    o = jnp.einsum('brhnqk,brhnkc->brhnqc', p.astype(vv.dtype), vv).astype(jnp.float32)
    o = o.reshape(B, dil, H, Lp, hd)[:, :, :, :L].transpose(0, 3, 1, 2, 4).reshape(B, S, H, hd)
    lse = lse.reshape(B, dil, H, Lp)[..., :L].transpose(0, 3, 1, 2).reshape(B, S, H)
    return o, lse


def pool_mixer(p, pool_w, pool_scale):
    B, S, _ = p.shape
    pf = p.astype(jnp.float32)
    c = jnp.cumsum(pf, axis=1)
    pos = jnp.arange(1, S + 1, dtype=jnp.int32)[None, :, None]
    outs = []
    for gi, w in enumerate(POOL_WINDOWS):
        sl = slice(gi * POOL_GROUP, (gi + 1) * POOL_GROUP)
        cg = c[..., sl]
        lagged = jnp.pad(cg, ((0, 0), (w, 0), (0, 0)))[:, :S]
        cnt = jnp.minimum(pos, w).astype(jnp.float32)
        pooled = (cg - lagged) / cnt - pf[..., sl]
        outs.append(pooled @ pool_w[gi].astype(jnp.float32))
    y = jnp.concatenate(outs, axis=-1) * pool_scale.astype(jnp.float32)
    return y.astype(p.dtype)


def even_mixer(h, w_in, pool_w, pool_scale, w_out):
    B, S, _ = h.shape
    proj = h @ w_in
    q, k, v, p = jnp.split(proj, [A_WIDTH, 2 * A_WIDTH, 3 * A_WIDTH], axis=-1)
    q, k, v = (t.reshape(B, S, A_HEADS, HEAD_DIM) for t in (q, k, v))
    outs, lses = [], []
    for window, dil in DILATED_CONFIGS:
        o, l = dilated_branch(q, k, v, window, dil)
        outs.append(o)
        lses.append(l)
    wts = jax.nn.softmax(jnp.stack(lses, axis=0), axis=0)
    a_out = jnp.einsum('gbsh,gbshc->bshc', wts, jnp.stack(outs, axis=0))
    a_out = a_out.astype(h.dtype).reshape(B, S, A_WIDTH)
    b_out = pool_mixer(p, pool_w, pool_scale)
    return (jnp.concatenate([a_out, b_out], axis=-1) @ w_out).astype(h.dtype)


def hgrn2_chunked(q, k, v, log_f):
    B, S, H, dk = q.shape
    dv = v.shape[-1]
    C = HGRN_CHUNK
    N = S // C

    def chunks(t):
        return t.astype(jnp.float32).reshape(B, N, C, H, t.shape[-1]).transpose(0, 3, 1, 2, 4)

    q, k, v, g = (chunks(t) for t in (q, k, v, log_f))
    b = jnp.cumsum(g, axis=3)
    b_mid = b[:, :, :, C // 2 - 1:C // 2]
    b_last = b[:, :, :, -1:]
    att = jnp.einsum('bhnik,bhnjk->bhnij', q * jnp.exp(b - b_mid), k * jnp.exp(b_mid - b))
    causal = jnp.tril(jnp.ones((C, C), dtype=bool))
    att = jnp.where(causal, att, 0.0)
    o_intra = jnp.einsum('bhnij,bhnjv->bhniv', att, v)
    u = jnp.einsum('bhnjk,bhnjv->bhnkv', k * jnp.exp(b_last - b), v)
    d = jnp.exp(b_last[:, :, :, 0])

    def step(state, xs):
        dn, un = xs
        return dn[..., None] * state + un, state

    init = jnp.zeros((B, H, dk, dv), jnp.float32)
    _, s_start = lax.scan(step, init, (jnp.moveaxis(d, 2, 0), jnp.moveaxis(u, 2, 0)))
    s_start = jnp.moveaxis(s_start, 0, 2)
    o_inter = jnp.einsum('bhnik,bhnkv->bhniv', q * jnp.exp(b), s_start)
    o = o_intra + o_inter
    return o.transpose(0, 2, 3, 1, 4).reshape(B, S, H, dv)


def hgrn2_mixer(h, w_in, lb, out_norm, w_out):
    B, S, _ = h.shape
    q, f_pre, i, gate = jnp.split(h @ w_in, 4, axis=-1)
    f = lb + (1.0 - lb) * jax.nn.sigmoid(f_pre.astype(jnp.float32))
    q = jax.nn.silu(q)

    def heads(t):
        return t.reshape(B, S, HGRN_HEADS, HGRN_HEAD_DIM)

    o = hgrn2_chunked(heads(q), heads(1.0 - f), heads(i), heads(jnp.log(f)))
    o = rms_norm(o, out_norm.reshape(HGRN_HEADS, HGRN_HEAD_DIM)).reshape(B, S, HGRN_WIDTH)
    o = o * jax.nn.silu(gate.astype(jnp.float32))
    return (o.astype(h.dtype) @ w_out).astype(h.dtype)


def swiglu(h, w_gate, w_up, w_down):
    return ((jax.nn.silu(h @ w_gate) * (h @ w_up)) @ w_down).astype(h.dtype)


def moe_swiglu(h, router_w, w_g, w_u, w_d):
    B, S, D = h.shape
    T = B * S
    xf = h.reshape(T, D)
    logits = (xf @ router_w).astype(jnp.float32)
    top_val, top_idx = lax.top_k(logits, TOP_K)
    gates = jax.nn.softmax(top_val, axis=-1)
    A = T * TOP_K
    e_flat = top_idx.reshape(A).astype(jnp.int32)
    tok = jnp.arange(A, dtype=jnp.int32) // TOP_K
    gate_flat = gates.reshape(A)
    order = jnp.argsort(e_flat)
    e_sorted = e_flat[order]
    counts = jnp.zeros((N_EXPERTS,), jnp.int32).at[e_flat].add(1)
    starts = jnp.cumsum(counts) - counts
    padded = (counts + MOE_BLOCK_ROWS - 1) // MOE_BLOCK_ROWS * MOE_BLOCK_ROWS
    pends = jnp.cumsum(padded)
    pstarts = pends - padded
    dest = pstarts[e_sorted] + jnp.arange(A, dtype=jnp.int32) - starts[e_sorted]
    NB = -(-A // MOE_BLOCK_ROWS) + N_EXPERTS
    P = NB * MOE_BLOCK_ROWS
    row_tok = jnp.full((P,), T, jnp.int32).at[dest].set(tok[order])
    row_gate = jnp.zeros((P,), jnp.float32).at[dest].set(gate_flat[order])
    xf_pad = jnp.concatenate([xf, jnp.zeros((1, D), xf.dtype)], axis=0)
    x_rows = xf_pad[row_tok].reshape(NB, MOE_BLOCK_ROWS, D)
    block_e = jnp.minimum(
        jnp.searchsorted(pends, jnp.arange(NB, dtype=jnp.int32) * MOE_BLOCK_ROWS, side='right'),
        N_EXPERTS - 1).astype(jnp.int32)

    def expert_block(args):
        xb, e = args
        return (jax.nn.silu(xb @ w_g[e]) * (xb @ w_u[e])) @ w_d[e]

    y_rows = lax.map(expert_block, (x_rows, block_e)).reshape(P, D)
    y_rows = y_rows.astype(jnp.float32) * row_gate[:, None]
    y = jnp.zeros((T + 1, D), jnp.float32).at[row_tok].add(y_rows)[:T]
    return y.reshape(B, S, D).astype(h.dtype)


def setup_inputs(seed: int = 0) -> dict:
    key = jax.random.key(seed)
    ks = jax.random.split(key, 20)

    def nrm(k, shape, scale):
        return jax.random.normal(k, shape, jnp.float32) * scale

    return {
        "x": nrm(ks[0], (BATCH, SEQ, D_MODEL), 1.0),
        "norm_mix": 1.0 + nrm(ks[1], (DEPTH, D_MODEL), 0.02),
        "norm_ffn": 1.0 + nrm(ks[2], (DEPTH, D_MODEL), 0.02),
        "w_in_even": nrm(ks[3], (N_EVEN, D_MODEL, EVEN_IN_WIDTH), D_MODEL ** -0.5),
        "pool_w": nrm(ks[4], (N_EVEN, len(POOL_WINDOWS), POOL_GROUP, POOL_GROUP), POOL_GROUP ** -0.5),
        "pool_scale": 1.0 + nrm(ks[5], (N_EVEN, B_WIDTH), 0.1),
        "w_out_even": nrm(ks[6], (N_EVEN, MIX_WIDTH, D_MODEL), MIX_WIDTH ** -0.5),
        "ffn_w_gate": nrm(ks[7], (N_EVEN, D_MODEL, D_FF), D_MODEL ** -0.5),
        "ffn_w_up": nrm(ks[8], (N_EVEN, D_MODEL, D_FF), D_MODEL ** -0.5),
        "ffn_w_down": nrm(ks[9], (N_EVEN, D_FF, D_MODEL), D_FF ** -0.5),
        "w_in_odd": nrm(ks[10], (N_ODD, D_MODEL, 4 * HGRN_WIDTH), D_MODEL ** -0.5),
        "lower_bound_logits": nrm(ks[11], (DEPTH, HGRN_WIDTH), 0.1),
        "hgrn_out_norm": 1.0 + nrm(ks[12], (N_ODD, HGRN_WIDTH), 0.02),
        "w_out_odd": nrm(ks[13], (N_ODD, HGRN_WIDTH, D_MODEL), HGRN_WIDTH ** -0.5),
        "router_w": nrm(ks[14], (N_ODD, D_MODEL, N_EXPERTS), D_MODEL ** -0.5),
        "moe_w_gate": nrm(ks[15], (N_ODD, N_EXPERTS, D_MODEL, D_EXPERT), D_MODEL ** -0.5),
        "moe_w_up": nrm(ks[16], (N_ODD, N_EXPERTS, D_MODEL, D_EXPERT), D_MODEL ** -0.5),
        "moe_w_down": nrm(ks[17], (N_ODD, N_EXPERTS, D_EXPERT, D_MODEL), D_EXPERT ** -0.5),
        "norm_final": 1.0 + nrm(ks[18], (D_MODEL,), 0.02),
    }


def reference(x, norm_mix, norm_ffn, w_in_even, pool_w, pool_scale, w_out_even,
              ffn_w_gate, ffn_w_up, ffn_w_down, w_in_odd, lower_bound_logits,
              hgrn_out_norm, w_out_odd, router_w, moe_w_gate, moe_w_up, moe_w_down,
              norm_final):
    lb_all = jnp.cumsum(jax.nn.softmax(lower_bound_logits.astype(jnp.float32), axis=0), axis=0)
    lb_all = lb_all - lb_all[:1]
    for layer in range(DEPTH):
        li = layer // 2
        h = rms_norm(x, norm_mix[layer])
        if layer % 2 == 0:
            x = x + even_mixer(h, w_in_even[li], pool_w[li], pool_scale[li], w_out_even[li])
            h = rms_norm(x, norm_ffn[layer])
            x = x + swiglu(h, ffn_w_gate[li], ffn_w_up[li], ffn_w_down[li])
        else:
            x = x + hgrn2_mixer(h, w_in_odd[li], lb_all[layer], hgrn_out_norm[li], w_out_odd[li])
            h = rms_norm(x, norm_ffn[layer])
            x = x + moe_swiglu(h, router_w[li], moe_w_gate[li], moe_w_up[li], moe_w_down[li])
    return rms_norm(x, norm_final)
```

```python
import numpy as np
from contextlib import ExitStack
import concourse.bass as bass
import concourse.mybir as mybir
from concourse.bass_utils import run_bass_kernel_spmd

F32 = mybir.dt.float32
BF16 = mybir.dt.bfloat16
AF = mybir.ActivationFunctionType
ALU = mybir.AluOpType
AX = mybir.AxisListType

S = 2048
D = 2048
NT = 16
EPS = 1e-6


class T:
    __slots__ = ('w', 'r')

    def __init__(s):
        s.w = None
        s.r = {}


def Ts(n):
    return [T() for _ in range(n)]


class Prog:
    ENG = ('pe', 'act', 'dve', 'pool', 'sp')

    def __init__(s, nc, es, ndma=32):
        s.nc = nc
        s.e = {'pe': nc.tensor, 'act': nc.scalar, 'dve': nc.vector, 'pool': nc.gpsimd, 'sp': nc.sync}
        s.sem = {k: es.enter_context(nc.semaphore('s_' + k)) for k in s.ENG}
        s.cnt = {k: 0 for k in s.ENG}
        s.waited = {k: {} for k in s.ENG}
        s.dsem = [es.enter_context(nc.semaphore('d%d' % i)) for i in range(ndma)]
        s.dcnt = [0] * ndma
        s.dnext = 0
        s.nins = {k: 0 for k in s.ENG}

    def _sem(s, k):
        return s.sem[k] if isinstance(k, str) else s.dsem[k]

    def _need(s, eng, waits, t):
        if t is None:
            return
        k, v = t
        if eng == 'pe' and k == 'pe':
            return
        if s.waited[eng].get(k, 0) >= v:
            return
        if waits.get(k, 0) < v:
            waits[k] = v

    def op(s, eng, fn, reads=(), writes=(), sig=True, dma=False):
        waits = {}
        for t in reads:
            s._need(eng, waits, t.w)
        for t in writes:
            s._need(eng, waits, t.w)
            for k, v in t.r.items():
                s._need(eng, waits, (k, v))
        if dma:
            k = s.dnext
            s.dnext = (s.dnext + 1) % len(s.dsem)
            s._need(eng, waits, (k, s.dcnt[k]))
            s.dcnt[k] += 16
            ticket = (k, s.dcnt[k])
        elif sig:
            s.cnt[eng] += 1
            ticket = (eng, s.cnt[eng])
        else:
            ticket = (eng, s.cnt[eng] + 1)
        E = s.e[eng]
        for k, v in waits.items():
            s.waited[eng][k] = v
            E.wait_ge(s._sem(k), v)
        ins = fn(E)
        s.nins[eng] += 1
        if dma:
            ins.then_inc(s.dsem[ticket[0]], 16)
        elif sig:
            ins.then_inc(s.sem[eng], 1)
        for t in reads:
            if t.r.get(ticket[0], 0) < ticket[1]:
                t.r[ticket[0]] = ticket[1]
        for t in writes:
            t.w = ticket
            t.r = {}
        return ticket

    def barrier(s):
        for eng in s.ENG:
            E = s.e[eng]
            for f in s.ENG:
                if f == eng or s.cnt[f] == 0:
                    continue
                if s.waited[eng].get(f, 0) < s.cnt[f]:
                    s.waited[eng][f] = s.cnt[f]
                    E.wait_ge(s.sem[f], s.cnt[f])
            for k in range(len(s.dsem)):
                if s.dcnt[k] and s.waited[eng].get(k, 0) < s.dcnt[k]:
                    s.waited[eng][k] = s.dcnt[k]
                    E.wait_ge(s.dsem[k], s.dcnt[k])


class Ctx:
    pass


_UID = [0]


def SB(nc, name, shape, dt):
    _UID[0] += 1
    return nc.sbuf_tensor('%s_u%d' % (name, _UID[0]), shape, dt)


def mm(P, out, lhsT, rhs, start, stop, reads, writes, sig=None):
    if sig is None:
        sig = stop
    return P.op('pe', lambda e: e.matmul(out, lhsT, rhs, start=start, stop=stop), reads, writes, sig=sig)


def evac_engine(i):
    return 'act' if (i & 1) else 'dve'


def copy_op(P, eng, out, in_, reads, writes):
    if eng == 'act':
        return P.op('act', lambda e: e.copy(out, in_), reads, writes)
    return P.op(eng, lambda e: e.tensor_copy(out, in_), reads, writes)


def phase_in(C):
    nc, P = C.nc, C.P
    with ExitStack() as es:
        xin = [es.enter_context(SB(nc, 'pi_xin%d' % i, [128, D], F32)) for i in range(2)]
        xin_t = Ts(2)
        stg = [es.enter_context(SB(nc, 'pi_stg%d' % i, [128, 16, 512], F32)) for i in range(2)]
        stg_t = Ts(2)
        k = 0
        for tg in range(4):
            sb, sb_t = stg[tg % 2], stg_t[tg % 2]
            for t4 in range(4):
                tt = tg * 4 + t4
                xb, xb_t = xin[tt % 2], xin_t[tt % 2]
                P.op('sp', lambda e: e.dma_start(out=xb[:], in_=C.x[tt * 128:(tt + 1) * 128, :]),
                     [], [xb_t], dma=True)
                for d4 in range(4):
                    ps, ps_t = C.psum()
                    for j in range(4):
                        dc = d4 * 4 + j
                        P.op('pe', lambda e: e.transpose(ps[:, j * 128:(j + 1) * 128],
                                                         xb[:, dc * 128:(dc + 1) * 128], C.ident_f[:]),
                             [xb_t, C.const_t], [ps_t], sig=(j == 3))
                    copy_op(P, evac_engine(k), sb[:, d4 * 4:(d4 + 1) * 4, t4 * 128:(t4 + 1) * 128],
                            ps[:].rearrange("p (c t) -> p c t", c=4), [ps_t], [sb_t])
                    k += 1
            P.op('sp', lambda e: e.dma_start(
                out=C.xT[:, tg * 512:(tg + 1) * 512].rearrange("(c p) t -> p c t", p=128), in_=sb[:]),
                [sb_t], [C.xT_t[dc][tg] for dc in range(16)], dma=True)
    P.barrier()


def load_norm(C, tg, gcol, X, X_t, sq, sq_t, rb, rb_t):
    nc, P = C.nc, C.P
    P.op('sp', lambda e: e.dma_start(out=X[:], in_=C.xT[:, tg * 512:(tg + 1) * 512].rearrange("(c p) t -> p c t", p=128)),
         [C.xT_t[dc][tg] for dc in range(16)], [X_t], dma=True)
    for h in range(2):
        P.op('act', lambda e: e.activation(out=sq[:, h * 8:(h + 1) * 8, :], in_=X[:, h * 8:(h + 1) * 8, :], func=AF.Square),
             [X_t], [sq_t[h]])
    ps, ps_t = C.psum()
    for dc in range(16):
        mm(P, ps[:], C.ones_b[:], sq[:, dc, :], dc == 0, dc == 15, [sq_t[dc // 8], C.const_t], [ps_t])
    P.op('act', lambda e: e.activation(out=rb[:], in_=ps[:], func=AF.Ln, scale=1.0 / D, bias=C.eps_col[:, 0:1]),
         [ps_t, C.const_t], [rb_t])
    P.op('act', lambda e: e.activation(out=rb[:], in_=rb[:], func=AF.Exp, scale=-0.5), [rb_t], [rb_t])


def phase_out(C):
    nc, P = C.nc, C.P
    with ExitStack() as es:
        X = [es.enter_context(SB(nc, 'po_X%d' % i, [128, 16, 512], F32)) for i in range(2)]
        X_t = Ts(2)
        sq = es.enter_context(SB(nc, 'po_sq', [128, 16, 512], BF16))
        sq_t = Ts(2)
        rb = es.enter_context(SB(nc, 'po_rb', [128, 512], F32))
        rb_t = T()
        og = [es.enter_context(SB(nc, 'po_o%d' % i, [128, D], F32)) for i in range(2)]
        og_t = Ts(2)
        k = 0
        for tg in range(4):
            Xb, Xb_t = X[tg % 2], X_t[tg % 2]
            load_norm(C, tg, None, Xb, Xb_t, sq, sq_t, rb, rb_t)
            for dc in range(16):
                P.op('dve', lambda e: e.scalar_tensor_tensor(out=Xb[:, dc, :], in0=Xb[:, dc, :], scalar=C.gfin[:, dc:dc + 1],
                                                           in1=rb[:], op0=ALU.mult, op1=ALU.mult),
                     [Xb_t, rb_t, C.const_t], [Xb_t])
            for t4 in range(4):
                tt = tg * 4 + t4
                ob, ob_t = og[tt % 2], og_t[tt % 2]
                for d4 in range(4):
                    ps, ps_t = C.psum()
                    for j in range(4):
                        dc = d4 * 4 + j
                        P.op('pe', lambda e: e.transpose(ps[:, j * 128:(j + 1) * 128],
                                                         Xb[:, dc, t4 * 128:(t4 + 1) * 128], C.ident_f[:]),
                             [Xb_t, C.const_t], [ps_t], sig=(j == 3))
                    copy_op(P, evac_engine(k), ob[:, d4 * 512:(d4 + 1) * 512], ps[:], [ps_t], [ob_t])
                    k += 1
                P.op('sp', lambda e: e.dma_start(out=C.out[tt * 128:(tt + 1) * 128, :], in_=ob[:]),
                     [ob_t], [C.out_t], dma=True)
    P.barrier()


class WPool:
    def __init__(s, C, es, name, nbuf, shape):
        s.C = C
        s.bufs = [es.enter_context(SB(C.nc, '%s%d' % (name, i), shape, BF16)) for i in range(nbuf)]
        s.ts = Ts(nbuf)
        s.i = 0

    def load(s, src_ap, view=None):
        i = s.i % len(s.bufs)
        s.i += 1
        b, t = s.bufs[i], s.ts[i]
        dst = b[:] if view is None else view(b)
        s.C.P.op('pool', lambda e: e.dma_start(out=dst, in_=src_ap), [], [t], dma=True)
        return b, t


def make_hT(C, g_col, hT, hT_t, tgs=(0, 1, 2, 3), router=None, nbuf=2):
    nc, P = C.nc, C.P
    with ExitStack() as es:
        X = [es.enter_context(SB(nc, 'nh_X%d' % i, [128, 16, 512], F32)) for i in range(nbuf)]
        X_t = Ts(nbuf)
        sq = es.enter_context(SB(nc, 'nh_sq', [128, 16, 512], BF16))
        sq_t = Ts(2)
        rb = [es.enter_context(SB(nc, 'nh_rb%d' % i, [128, 512], F32)) for i in range(2)]
        rb_t = Ts(2)
        for i, tg in enumerate(tgs):
            Xb, Xb_t = X[i % nbuf], X_t[i % nbuf]
            load_norm(C, tg, None, Xb, Xb_t, sq, sq_t, rb[i % 2], rb_t[i % 2])
            if router is not None:
                router(tg, Xb, Xb_t, rb[i % 2], rb_t[i % 2])
            for dc in range(16):
                P.op('dve', lambda e: e.scalar_tensor_tensor(out=hT[:, dc, i * 512:(i + 1) * 512], in0=Xb[:, dc, :],
                                                           scalar=g_col[:, dc:dc + 1], in1=rb[i % 2][:],
                                                           op0=ALU.mult, op1=ALU.mult),
                     [Xb_t, rb_t[i % 2], C.const_t], [hT_t[i]])
    P.barrier()


def residual_out(C, dchunk, tg, ps, ps_t, rpool):
    P = C.P
    b, b_t = rpool()
    reg = C.xT_t[dchunk][tg]
    ap = C.xT[dchunk * 128:(dchunk + 1) * 128, tg * 512:(tg + 1) * 512]
    P.op('sp', lambda e: e.dma_start(out=b[:], in_=ap), [reg], [b_t], dma=True)
    P.op('dve', lambda e: e.tensor_tensor(out=b[:], in0=ps[:], in1=b[:], op=ALU.add), [ps_t, b_t], [b_t])
    P.op('sp', lambda e: e.dma_start(out=ap, in_=b[:]), [b_t], [reg], dma=True)


class ResPipe:
    def __init__(s, C, es, name, n, order):
        s.C = C
        s.n = n
        s.bufs = [es.enter_context(SB(C.nc, '%s%d' % (name, i), [128, 512], F32)) for i in range(n)]
        s.ts = Ts(n)
        s.order = list(order)
        s.issued = 0
        s.done = 0

    def _ap(s, i):
        dchunk, tg = s.order[i]
        return s.C.xT[dchunk * 128:(dchunk + 1) * 128, tg * 512:(tg + 1) * 512], s.C.xT_t[dchunk][tg]

    def _issue(s):
        i = s.issued
        b, b_t = s.bufs[i % s.n], s.ts[i % s.n]
        ap, reg = s._ap(i)
        s.C.P.op('sp', lambda e: e.dma_start(out=b[:], in_=ap), [reg], [b_t], dma=True)
        s.issued += 1

    def finish(s, ps, ps_t):
        i = s.done
        while s.issued < min(len(s.order), i + s.n - 1) or s.issued <= i:
            s._issue()
        b, b_t = s.bufs[i % s.n], s.ts[i % s.n]
        ap, reg = s._ap(i)
        P = s.C.P
        P.op('dve', lambda e: e.tensor_tensor(out=b[:], in0=ps[:], in1=b[:], op=ALU.add), [ps_t, b_t], [b_t])
        P.op('sp', lambda e: e.dma_start(out=ap, in_=b[:]), [b_t], [reg], dma=True)
        s.done += 1


def make_rpool(C, es, name, n=4):
    bufs = [es.enter_context(SB(C.nc, '%s%d' % (name, i), [128, 512], F32)) for i in range(n)]
    ts = Ts(n)
    st = [0]

    def get():
        i = st[0] % n
        st[0] += 1
        return bufs[i], ts[i]
    return get


def out_proj_residual(C, es, A, A_t, W, wpool, rpool):
    P = C.P
    rp = ResPipe(C, es, 'opr', 12, [(wt * 2 + m, tg) for wt in range(8) for m in range(2) for tg in range(4)])
    for wt in range(8):
        wb, wb_t = wpool.load(W[:, wt * 256:(wt + 1) * 256].rearrange("(c p) n -> p c n", p=128))
        for m in range(2):
            for tg in range(4):
                ps, ps_t = C.psum('r')
                for fc in range(16):
                    mm(P, ps[:], wb[:, fc, m * 128:(m + 1) * 128], A[:, fc, tg * 512:(tg + 1) * 512],
                       fc == 0, fc == 15, [wb_t, A_t[tg]], [ps_t])
                rp.finish(ps, ps_t)


def phase_mix0(C):
    nc, P = C.nc, C.P
    aT = C.aT_scr
    aT_t = C.aT_scr_t
    with ExitStack() as es0:
        hT = es0.enter_context(SB(nc, 'm0_hT', [128, 16, 2048], BF16))
        hT_t = Ts(4)
        gcol = es0.enter_context(SB(nc, 'm0_g', [128, 16], F32))
        P.op('sp', lambda e: e.dma_start(out=gcol[:], in_=C.norm_mix[0, :].rearrange("(c p) -> p c", p=128),
                                         allow_slow_non_contiguous=True), [], [C.const_t], dma=True)
        make_hT(C, gcol, hT, hT_t)
        with ExitStack() as es:
            wpool = WPool(C, es, 'm0_w', 5, [128, 16, 256])
            QT = [es.enter_context(SB(nc, 'm0_QT', [128, 2, 2048], BF16)) for _ in range(2)]
            KT = [es.enter_context(SB(nc, 'm0_KT', [128, 2, 2048], BF16)) for _ in range(2)]
            V = [es.enter_context(SB(nc, 'm0_V', [128, 16, 256], BF16)) for _ in range(2)]
            QT_t, KT_t, V_t = [Ts(2), Ts(2)], [Ts(2), Ts(2)], Ts(2)
            MT = es.enter_context(SB(nc, 'm0_MT', [128, 2176], BF16))
            P.op('pool', lambda e: e.dma_start(out=MT[:], in_=C.c_MT[:, :]), [], [C.const_t], dma=True)
            NE = 6
            Eb = [es.enter_context(SB(nc, 'm0_E%d' % i, [128, 512], BF16)) for i in range(NE)]
            Eb_t = Ts(NE)
            rc = es.enter_context(SB(nc, 'm0_rc', [128, 512], F32))
            rc_t = T()
            ah = [es.enter_context(SB(nc, 'm0_ah%d' % i, [128, 2048], BF16)) for i in range(2)]
            ah_t = Ts(2)
            st = {'ecnt': 0, 'hcnt': 0, 'k': 0}

            def proj_steps(g):
                gb = g % 2
                wq, wq_t = wpool.load(C.w_in_even[:, g * 256:(g + 1) * 256].rearrange("(c p) n -> p c n", p=128))
                wk, wk_t = wpool.load(C.w_in_even[:, 1536 + g * 256:1536 + (g + 1) * 256].rearrange("(c p) n -> p c n", p=128))
                wv, wv_t = wpool.load(C.w_in_even[:, 3072 + g * 256:3072 + (g + 1) * 256].rearrange("(c p) n -> p c n", p=128))
                for (wb, wb_t, dst, dst_t, scale) in ((wq, wq_t, QT[gb], QT_t[gb], 128.0 ** -0.5), (wk, wk_t, KT[gb], KT_t[gb], 1.0)):
                    for hh in range(2):
                        for tg in range(4):
                            ps, ps_t = C.psum('r')
                            for dc in range(16):
                                mm(P, ps[:], wb[:, dc, hh * 128:(hh + 1) * 128], hT[:, dc, tg * 512:(tg + 1) * 512],
                                   dc == 0, dc == 15, [wb_t, hT_t[tg]], [ps_t])
                            if st['k'] % 2 == 0:
                                P.op('act', lambda e: e.activation(out=dst[:, hh, tg * 512:(tg + 1) * 512], in_=ps[:],
                                                                   func=AF.Copy, scale=scale), [ps_t], [dst_t[hh]])
                            else:
                                P.op('dve', lambda e: e.tensor_scalar_mul(out=dst[:, hh, tg * 512:(tg + 1) * 512], in0=ps[:],
                                                                          scalar1=scale), [ps_t], [dst_t[hh]])
                            st['k'] += 1
                            yield
                for tt in range(16):
                    ps, ps_t = C.psum('r')
                    for dc in range(16):
                        mm(P, ps[:, 0:256], hT[:, dc, tt * 128:(tt + 1) * 128], wv[:, dc, :], dc == 0, dc == 15,
                           [wv_t, hT_t[tt // 4]], [ps_t])
                    copy_op(P, evac_engine(tt), V[gb][:, tt, :], ps[:, 0:256], [ps_t], [V_t[gb]])
                    yield

            def attn_steps(g):
                gb = g % 2
                for hh in range(2):
                    head = g * 2 + hh
                    ab, ab_t = ah[st['hcnt'] % 2], ah_t[st['hcnt'] % 2]
                    st['hcnt'] += 1
                    for qg in range(4):
                        psO, psO_t = C.psum('a')
                        psD, psD_t = C.psum('b')
                        nk = 4 * qg + 4
                        pend = None
                        for kt in range(nk + 1):
                            cur = None
                            if kt < nk:
                                q0 = max(512 * qg, 128 * kt)
                                N = 512 * qg + 512 - q0
                                ps, ps_t = C.psum('r')
                                mm(P, ps[:, 0:N], KT[gb][:, hh, kt * 128:(kt + 1) * 128], QT[gb][:, hh, q0:q0 + N], True, True,
                                   [KT_t[gb][hh], QT_t[gb][hh]], [ps_t])
                                E, E_t = Eb[st['ecnt'] % NE], Eb_t[st['ecnt'] % NE]
                                st['ecnt'] += 1
                                P.op('act', lambda e: e.activation(out=E[:, 0:N], in_=ps[:, 0:N], func=AF.Exp), [ps_t], [E_t])
                                d0 = q0 - 128 * kt
                                P.op('dve', lambda e: e.tensor_tensor(out=E[:, 0:N], in0=E[:, 0:N], in1=MT[:, d0:d0 + N], op=ALU.mult),
                                     [E_t, C.const_t], [E_t])
                                cur = (kt, q0, N, E, E_t)
                            if pend is not None:
                                pk, pq0, pN, pE, pE_t = pend
                                c0 = pq0 - 512 * qg
                                mm(P, psO[:, c0:c0 + pN], V[gb][:, pk, hh * 128:(hh + 1) * 128], pE[:, 0:pN], pk == 0, pk == nk - 1,
                                   [V_t[gb], pE_t], [psO_t])
                                mm(P, psD[:, c0:c0 + pN], C.ones_b[:], pE[:, 0:pN], pk == 0, pk == nk - 1,
                                   [C.const_t, pE_t], [psD_t])
                            pend = cur
                            yield
                        P.op('dve', lambda e: e.reciprocal(out=rc[:], in_=psD[:]), [psD_t], [rc_t])
                        P.op('dve', lambda e: e.tensor_tensor(out=ab[:, qg * 512:(qg + 1) * 512], in0=psO[:], in1=rc[:], op=ALU.mult),
                             [psO_t, rc_t], [ab_t])
                    P.op('sp', lambda e: e.dma_start(out=aT[head * 128:(head + 1) * 128, :], in_=ab[:]), [ab_t], [aT_t[head]], dma=True)

            pw = es.enter_context(SB(nc, 'm0_pw', [128, 4, 128], BF16))
            psc = es.enter_context(SB(nc, 'm0_psc', [128, 4], F32))
            rct = es.enter_context(SB(nc, 'm0_rct', [128, 4, 16], F32))
            P.op('pool', lambda e: e.dma_start(out=pw[:], in_=C.pool_w.rearrange("g i o -> i g o")), [], [C.const_t], dma=True)
            P.op('sp', lambda e: e.dma_start(out=psc[:], in_=C.pool_scale.rearrange("(g p) -> p g", p=128),
                                             allow_slow_non_contiguous=True), [], [C.const_t], dma=True)
            P.op('sp', lambda e: e.dma_start(out=rct[:], in_=C.c_rc[:, :, :]), [], [C.const_t], dma=True)
            pb = [es.enter_context(SB(nc, 'm0_pb%d' % i, [128, 16 + 2048], F32)) for i in range(3)]
            pb_t = Ts(3)
            for i in range(3):
                P.op('pool', lambda e: e.memset(pb[i][:, 0:16], 0.0), [], [pb_t[i]])
            t16 = es.enter_context(SB(nc, 'm0_t16', [128, 16], F32))
            t16_t = T()
            pa = [es.enter_context(SB(nc, 'm0_pa%d' % i, [128, 2048], BF16)) for i in range(2)]
            pa_t = Ts(2)

            def pool_steps():
                wp = wp_t = None
                for gi in range(4):
                    if gi % 2 == 0:
                        wp, wp_t = wpool.load(C.w_in_even[:, 4608 + (gi // 2) * 256:4608 + (gi // 2 + 1) * 256].rearrange("(c p) n -> p c n", p=128))
                    w = 2 << gi
                    p0, p0_t = pb[0], pb_t[0]
                    for tg in range(4):
                        ps, ps_t = C.psum('r')
                        for dc in range(16):
                            mm(P, ps[:], wp[:, dc, (gi % 2) * 128:(gi % 2 + 1) * 128], hT[:, dc, tg * 512:(tg + 1) * 512],
                               dc == 0, dc == 15, [wp_t, hT_t[tg]], [ps_t])
                        copy_op(P, evac_engine(tg), p0[:, 16 + tg * 512:16 + (tg + 1) * 512], ps[:], [ps_t], [p0_t])
                        yield
                    src, src_t = p0, p0_t
                    step = 1
                    alt = 1
                    while step < w:
                        dst, dst_t = pb[alt], pb_t[alt]
                        P.op('dve', lambda e: e.tensor_tensor(out=dst[:, 16:16 + 2048], in0=src[:, 16:16 + 2048],
                                                              in1=src[:, 16 - step:16 - step + 2048], op=ALU.add),
                             [src_t], [dst_t])
                        src, src_t = dst, dst_t
                        alt = 3 - alt
                        step *= 2
                        yield
                    ab, ab_t = pa[0], pa_t[0]
                    P.op('dve', lambda e: e.scalar_tensor_tensor(out=ab[:], in0=src[:, 16:16 + 2048], scalar=1.0 / w,
                                                               in1=p0[:, 16:16 + 2048], op0=ALU.mult, op1=ALU.subtract),
                         [src_t, p0_t], [ab_t])
                    yield
                    P.op('dve', lambda e: e.tensor_tensor(out=t16[:], in0=src[:, 16:32], in1=rct[:, gi, :], op=ALU.mult),
                         [src_t, C.const_t], [t16_t])
                    P.op('dve', lambda e: e.tensor_tensor(out=ab[:, 0:16], in0=t16[:], in1=p0[:, 16:32], op=ALU.subtract),
                         [t16_t, p0_t], [ab_t])
                    yield
                    ob, ob_t = pa[1], pa_t[1]
                    for tg in range(4):
                        ps, ps_t = C.psum('r')
                        mm(P, ps[:], pw[:, gi, :], ab[:, tg * 512:(tg + 1) * 512], True, True, [C.const_t, ab_t], [ps_t])
                        P.op('act', lambda e: e.activation(out=ob[:, tg * 512:(tg + 1) * 512], in_=ps[:], func=AF.Copy,
                                                           scale=psc[:, gi:gi + 1]), [ps_t, C.const_t], [ob_t])
                        yield
                    P.op('sp', lambda e: e.dma_start(out=aT[(12 + gi) * 128:(13 + gi) * 128, :], in_=ob[:]), [ob_t], [aT_t[12 + gi]], dma=True)
                    yield

            for _ in proj_steps(0):
                pass
            for g in range(6):
                pg = proj_steps(g + 1) if g + 1 < 6 else pool_steps()
                every = 3 if g + 1 < 6 else 1
                i = 0
                for _ in attn_steps(g):
                    i += 1
                    if pg is not None and i % every == 0:
                        if next(pg, 'end') == 'end':
                            pg = None
                if pg is not None:
                    for _ in pg:
                        pass
        P.barrier()
    with ExitStack() as es:
        A = es.enter_context(SB(nc, 'm0_A', [128, 16, 2048], BF16))
        A_t = Ts(4)
        for fc in range(16):
            P.op('sp', lambda e: e.dma_start(out=A[:, fc, :], in_=aT[fc * 128:(fc + 1) * 128, :]), [aT_t[fc]], A_t, dma=True)
        wpool = WPool(C, es, 'm0_wo', 3, [128, 16, 256])
        rpool = make_rpool(C, es, 'm0_r', 4)
        out_proj_residual(C, es, A, A_t, C.w_out_even, wpool, rpool)
    P.barrier()


def swiglu_A(C, XT, XT_t, groups, Wg, Wu, F, AT, AT_t, wpool, sgpool):
    P = C.P
    k = 0
    for ft in range(F // 256):
        wg, wg_t = wpool.load(Wg[:, ft * 256:(ft + 1) * 256].rearrange("(c p) n -> p c n", p=128))
        wu, wu_t = wpool.load(Wu[:, ft * 256:(ft + 1) * 256].rearrange("(c p) n -> p c n", p=128))
        for m in range(2):
            ffc = ft * 2 + m
            for (c0, n, ti) in groups:
                psG, psG_t = C.psum('r')
                for dc in range(16):
                    mm(P, psG[:, 0:n], wg[:, dc, m * 128:(m + 1) * 128], XT[:, dc, c0:c0 + n], dc == 0, dc == 15,
                       [wg_t, XT_t[ti]], [psG_t])
                psU, psU_t = C.psum('a' if k % 2 == 0 else 'b')
                for dc in range(16):
                    mm(P, psU[:, 0:n], wu[:, dc, m * 128:(m + 1) * 128], XT[:, dc, c0:c0 + n], dc == 0, dc == 15,
                       [wu_t, XT_t[ti]], [psU_t])
                sg, sg_t = sgpool()
                P.op('act', lambda e: e.activation(out=sg[:, 0:n], in_=psG[:, 0:n], func=AF.Silu), [psG_t], [sg_t])
                P.op('dve', lambda e: e.tensor_tensor(out=AT[:, ffc, c0:c0 + n], in0=psU[:, 0:n], in1=sg[:, 0:n], op=ALU.mult),
                     [psU_t, sg_t], [AT_t[ti]])
                k += 1


def down_fm(C, AT, AT_t, groups, Wd, F, wpool, out_fn):
    P = C.P
    nf = F // 128
    for dt in range(8):
        wd, wd_t = wpool.load(Wd[:, dt * 256:(dt + 1) * 256].rearrange("(c p) n -> p c n", p=128))
        for m in range(2):
            for gi, (c0, n, ti) in enumerate(groups):
                ps, ps_t = C.psum('r')
                for fc in range(nf):
                    mm(P, ps[:, 0:n], wd[:, fc, m * 128:(m + 1) * 128], AT[:, fc, c0:c0 + n], fc == 0, fc == nf - 1,
                       [wd_t, AT_t[ti]], [ps_t])
                out_fn(dt * 2 + m, gi, ps, ps_t)


def make_sgpool(C, es, name, n=3, dt=F32):
    bufs = [es.enter_context(SB(C.nc, '%s%d' % (name, i), [128, 512], dt)) for i in range(n)]
    ts = Ts(n)
    st = [0]

    def get():
        i = st[0] % n
        st[0] += 1
        return bufs[i], ts[i]
    return get


def phase_ffn0(C):
    nc, P = C.nc, C.P
    F = 5632
    with ExitStack() as es0:
        gcol = es0.enter_context(SB(nc, 'f0_g', [128, 16], F32))
        P.op('sp', lambda e: e.dma_start(out=gcol[:], in_=C.norm_ffn[0, :].rearrange("(c p) -> p c", p=128),
                                         allow_slow_non_contiguous=True), [], [C.const_t], dma=True)
        hT = es0.enter_context(SB(nc, 'f0_hT', [128, 16, 1024], BF16))
        AT = es0.enter_context(SB(nc, 'f0_AT', [128, F // 128, 1024], BF16))
        for job in range(2):
            hT_t = Ts(2)
            AT_t = Ts(2)
            make_hT(C, gcol, hT, hT_t, tgs=(2 * job, 2 * job + 1))
            groups = [(0, 512, 0), (512, 512, 1)]
            with ExitStack() as es:
                wpool = WPool(C, es, 'f0_w', 4, [128, 16, 256])
                sgpool = make_sgpool(C, es, 'f0_sg', 3)
                swiglu_A(C, hT, hT_t, groups, C.ffn_w_gate, C.ffn_w_up, F, AT, AT_t, wpool, sgpool)
                P.barrier()
            with ExitStack() as es:
                wpool = WPool(C, es, 'f0_wd', 2, [128, F // 128, 256])
                rp = ResPipe(C, es, 'f0_r', 8, [(dt * 2 + m, 2 * job + gi) for dt in range(8) for m in range(2) for gi in range(2)])
                down_fm(C, AT, AT_t, groups, C.ffn_w_down, F, wpool,
                        lambda dchunk, gi, ps, ps_t: rp.finish(ps, ps_t))
                P.barrier()


def load_cols(C, dst, src_1d):
    C.P.op('sp', lambda e: e.dma_start(out=dst[:], in_=src_1d.rearrange("(c p) -> p c", p=128),
                                       allow_slow_non_contiguous=True), [], [C.const_t], dma=True)


def phase_mix1(C):
    nc, P = C.nc, C.P
    aT = C.aT_scr
    aT_t = C.aT_scr_t
    with ExitStack() as es0:
        hT = es0.enter_context(SB(nc, 'm1_hT', [128, 16, 2048], BF16))
        hT_t = Ts(4)
        gcol = es0.enter_context(SB(nc, 'm1_g', [128, 16], F32))
        ocol = es0.enter_context(SB(nc, 'm1_oc', [128, 16], F32))
        lb = es0.enter_context(SB(nc, 'm1_lb', [128, 16], F32))
        oml = es0.enter_context(SB(nc, 'm1_oml', [128, 16], F32))
        l0 = es0.enter_context(SB(nc, 'm1_l0', [128, 16], F32))
        load_cols(C, gcol, C.norm_mix[1, :])
        load_cols(C, ocol, C.hgrn_out_norm)
        load_cols(C, l0, C.lower_bound_logits[0, :])
        load_cols(C, lb, C.lower_bound_logits[1, :])
        P.barrier()
        P.op('dve', lambda e: e.tensor_tensor(out=lb[:], in0=lb[:], in1=l0[:], op=ALU.subtract), [C.const_t], [C.const_t])
        P.op('act', lambda e: e.activation(out=lb[:], in_=lb[:], func=AF.Sigmoid), [C.const_t], [C.const_t])
        P.op('dve', lambda e: e.tensor_scalar(out=oml[:], in0=lb[:], scalar1=-1.0, scalar2=1.0, op0=ALU.mult, op1=ALU.add),
             [C.const_t], [C.const_t])
        P.barrier()
        make_hT(C, gcol, hT, hT_t)
        with ExitStack() as es:
            wpool = WPool(C, es, 'm1_w', 4, [128, 16, 256])
            V = es.enter_context(SB(nc, 'm1_V', [128, 16, 256], BF16))
            V_t = T()
            B = [es.enter_context(SB(nc, 'm1_B%d' % i, [128, 2048], F32)) for i in range(4)]
            B_t = Ts(4)
            rst = es.enter_context(SB(nc, 'm1_rst', [128, 2048], F32))
            bd = es.enter_context(SB(nc, 'm1_bd', [128, 128], BF16))
            P.op('sp', lambda e: e.dma_start(out=rst[:], in_=C.c_rst[:, :]), [], [C.const_t], dma=True)
            P.op('pool', lambda e: e.dma_start(out=bd[:], in_=C.c_bd[:, :]), [], [C.const_t], dma=True)
            class HB:
                pass
            H = []
            for i in range(2):
                hb = HB()
                hb.KbT = es.enter_context(SB(nc, 'm1_KbT', [128, 2048], BF16))
                hb.QhT = es.enter_context(SB(nc, 'm1_QhT', [128, 2048], BF16))
                hb.KbTok = es.enter_context(SB(nc, 'm1_KbTok', [128, 16, 128], BF16))
                hb.SG = es.enter_context(SB(nc, 'm1_SG', [128, 2048], BF16))
                hb.Osb = es.enter_context(SB(nc, 'm1_O', [128, 2048], F32))
                hb.dn = es.enter_context(SB(nc, 'm1_dn', [128, 32], F32))
                hb.Sst = es.enter_context(SB(nc, 'm1_S', [128, 128], F32))
                hb.Sp = es.enter_context(SB(nc, 'm1_Sp', [128, 128], F32))
                hb.Sbf = es.enter_context(SB(nc, 'm1_Sbf', [128, 128], BF16))
                hb.attm = [es.enter_context(SB(nc, 'm1_att%d' % j, [128, 128], BF16)) for j in range(2)]
                hb.attm_t = Ts(2)
                (hb.KbT_t, hb.QhT_t, hb.KbTok_t, hb.SG_t, hb.Osb_t, hb.dn_t, hb.S_t, hb.Sp_t, hb.Sbf_t) = (T() for _ in range(9))
                H.append(hb)
            osq = es.enter_context(SB(nc, 'm1_osq', [128, 2048], BF16))
            orb = es.enter_context(SB(nc, 'm1_orb', [128, 512], F32))
            of = [es.enter_context(SB(nc, 'm1_of%d' % i, [128, 2048], BF16)) for i in range(1)] * 2
            of_t = Ts(1) * 2
            osq_t, orb_t = T(), T()
            eps128 = C.eps_col
            for g in range(8):
                wq, wq_t = wpool.load(C.w_in_odd[:, g * 256:(g + 1) * 256].rearrange("(c p) n -> p c n", p=128))
                wf, wf_t = wpool.load(C.w_in_odd[:, 2048 + g * 256:2048 + (g + 1) * 256].rearrange("(c p) n -> p c n", p=128))
                wi, wi_t = wpool.load(C.w_in_odd[:, 4096 + g * 256:4096 + (g + 1) * 256].rearrange("(c p) n -> p c n", p=128))
                wg, wg_t = wpool.load(C.w_in_odd[:, 6144 + g * 256:6144 + (g + 1) * 256].rearrange("(c p) n -> p c n", p=128))
                for tt in range(16):
                    ps, ps_t = C.psum('all')
                    for dc in range(16):
                        mm(P, ps[:, 0:256], hT[:, dc, tt * 128:(tt + 1) * 128], wi[:, dc, :], dc == 0, dc == 15,
                           [wi_t, hT_t[tt // 4]], [ps_t])
                    copy_op(P, evac_engine(tt), V[:, tt, :], ps[:, 0:256], [ps_t], [V_t])
                for hh in range(2):
                    h = 2 * g + hh
                    hb = H[hh]
                    hs = slice(hh * 128, (hh + 1) * 128)

                    def proj(wb, wb_t, func, dst, dst_t):
                        for tg in range(4):
                            ps, ps_t = C.psum('all')
                            for dc in range(16):
                                mm(P, ps[:], wb[:, dc, hs], hT[:, dc, tg * 512:(tg + 1) * 512], dc == 0, dc == 15,
                                   [wb_t, hT_t[tg]], [ps_t])
                            P.op('act', lambda e: e.activation(out=dst[:, tg * 512:(tg + 1) * 512], in_=ps[:], func=func),
                                 [ps_t], [dst_t])
                    proj(wf, wf_t, AF.Sigmoid, B[0], B_t[0])
                    P.op('dve', lambda e: e.tensor_scalar(out=B[0][:], in0=B[0][:], scalar1=oml[:, h:h + 1], scalar2=lb[:, h:h + 1],
                                                          op0=ALU.mult, op1=ALU.add), [B_t[0], C.const_t], [B_t[0]])
                    P.op('act', lambda e: e.activation(out=B[1][:], in_=B[0][:], func=AF.Ln), [B_t[0]], [B_t[1]])
                    P.op('dve', lambda e: e.tensor_tensor_scan(out=B[2][:], data0=rst[:], data1=B[1][:], initial=0.0,
                                                              op0=ALU.mult, op1=ALU.add), [B_t[1], C.const_t], [B_t[2]])
                    P.op('act', lambda e: e.activation(out=B[1][:], in_=B[2][:], func=AF.Exp), [B_t[2]], [B_t[1]])
                    P.op('act', lambda e: e.activation(out=B[3][:], in_=B[2][:], func=AF.Exp, scale=-1.0), [B_t[2]], [B_t[3]])
                    P.op('dve', lambda e: e.tensor_scalar(out=B[0][:], in0=B[0][:], scalar1=-1.0, scalar2=1.0, op0=ALU.mult, op1=ALU.add),
                         [B_t[0]], [B_t[0]])
                    P.op('dve', lambda e: e.tensor_tensor(out=hb.KbT[:], in0=B[0][:], in1=B[3][:], op=ALU.mult), [B_t[0], B_t[3]], [hb.KbT_t])
                    P.op('dve', lambda e: e.tensor_copy(hb.dn[:, 0:16], B[1][:].rearrange("p (n c) -> p n c", c=128)[:, :, 127]), [B_t[1]], [hb.dn_t])
                    proj(wq, wq_t, AF.Silu, B[2], B_t[2])
                    P.op('dve', lambda e: e.tensor_tensor(out=hb.QhT[:], in0=B[2][:], in1=B[1][:], op=ALU.mult), [B_t[2], B_t[1]], [hb.QhT_t])
                    proj(wg, wg_t, AF.Silu, hb.SG, hb.SG_t)
                    for t4 in range(4):
                        ps, ps_t = C.psum('all')
                        psb = ps[:].bitcast(BF16)
                        for j in range(4):
                            tt = t4 * 4 + j
                            P.op('pe', lambda e: e.transpose(psb[:, j * 128:(j + 1) * 128], hb.KbT[:, tt * 128:(tt + 1) * 128], C.ident_b[:]),
                                 [hb.KbT_t, C.const_t], [ps_t], sig=(j == 3))
                        copy_op(P, evac_engine(t4), hb.KbTok[:, t4 * 4:(t4 + 1) * 4, :], psb[:, 0:512].rearrange("p (c t) -> p c t", c=4),
                                [ps_t], [hb.KbTok_t])
                    P.op('pool', lambda e: e.memset(hb.Sst[:], 0.0), [], [hb.S_t])
                    P.op('pool', lambda e: e.memset(hb.Sbf[:], 0.0), [], [hb.Sbf_t])
                for m in range(16):
                    ms = slice(m * 128, (m + 1) * 128)
                    for hh in range(2):
                        hb = H[hh]
                        hs = slice(hh * 128, (hh + 1) * 128)
                        psA, psA_t = C.psum('r')
                        mm(P, psA[:, 0:128], hb.KbT[:, ms], hb.QhT[:, ms], True, True, [hb.KbT_t, hb.QhT_t], [psA_t])
                        am, am_t = hb.attm[m % 2], hb.attm_t[m % 2]
                        P.op('dve', lambda e: e.tensor_tensor(out=am[:], in0=psA[:, 0:128], in1=bd[:], op=ALU.mult),
                             [psA_t, C.const_t], [am_t])
                        psO, psO_t = C.psum('a')
                        mm(P, psO[:, 0:128], V[:, m, hs], am[:], True, False, [V_t, am_t], [psO_t], sig=False)
                        mm(P, psO[:, 0:128], hb.Sbf[:], hb.QhT[:, ms], False, True, [hb.Sbf_t, hb.QhT_t], [psO_t], sig=True)
                        if m < 15:
                            psU, psU_t = C.psum('b')
                            mm(P, psU[:, 0:128], hb.KbTok[:, m, :], V[:, m, hs], True, True, [hb.KbTok_t, V_t], [psU_t])
                            P.op('dve', lambda e: e.tensor_tensor(out=hb.Sp[:], in0=psU[:, 0:128], in1=hb.Sst[:], op=ALU.add),
                                 [psU_t, hb.S_t], [hb.Sp_t])
                            P.op('act', lambda e: e.activation(out=hb.Sbf[:], in_=hb.Sp[:], func=AF.Copy, scale=hb.dn[:, m:m + 1]),
                                 [hb.Sp_t, hb.dn_t], [hb.Sbf_t])
                            P.op('dve', lambda e: e.tensor_scalar_mul(out=hb.Sst[:], in0=hb.Sp[:], scalar1=hb.dn[:, m:m + 1]),
                                 [hb.Sp_t, hb.dn_t], [hb.S_t])
                        copy_op(P, 'act', hb.Osb[:, ms], psO[:, 0:128], [psO_t], [hb.Osb_t])
                for hh in range(2):
                    h = 2 * g + hh
                    hb = H[hh]
                    P.op('act', lambda e: e.activation(out=osq[:], in_=hb.Osb[:], func=AF.Square), [hb.Osb_t], [osq_t])
                    ob, ob_t = of[h % 2], of_t[h % 2]
                    for tg in range(4):
                        ts_ = slice(tg * 512, (tg + 1) * 512)
                        ps, ps_t = C.psum('r')
                        mm(P, ps[:], C.ones_b[:], osq[:, ts_], True, True, [C.const_t, osq_t], [ps_t])
                        P.op('act', lambda e: e.activation(out=orb[:], in_=ps[:], func=AF.Ln, scale=1.0 / 128, bias=eps128[:, 0:1]),
                             [ps_t, C.const_t], [orb_t])
                        P.op('act', lambda e: e.activation(out=orb[:], in_=orb[:], func=AF.Exp, scale=-0.5), [orb_t], [orb_t])
                        P.op('dve', lambda e: e.scalar_tensor_tensor(out=hb.Osb[:, ts_], in0=hb.Osb[:, ts_], scalar=ocol[:, h:h + 1], in1=orb[:],
                                                                   op0=ALU.mult, op1=ALU.mult), [hb.Osb_t, orb_t, C.const_t], [hb.Osb_t])
                        P.op('dve', lambda e: e.tensor_tensor(out=ob[:, ts_], in0=hb.Osb[:, ts_], in1=hb.SG[:, ts_], op=ALU.mult),
                             [hb.Osb_t, hb.SG_t], [ob_t])
                    P.op('sp', lambda e: e.dma_start(out=aT[h * 128:(h + 1) * 128, :], in_=ob[:]), [ob_t], [aT_t[h]], dma=True)
        P.barrier()
    with ExitStack() as es:
        A = es.enter_context(SB(nc, 'm1_A', [128, 16, 2048], BF16))
        A_t = Ts(4)
        for fc in range(16):
            P.op('sp', lambda e: e.dma_start(out=A[:, fc, :], in_=aT[fc * 128:(fc + 1) * 128, :]), [aT_t[fc]], A_t, dma=True)
        wpool = WPool(C, es, 'm1_wo', 3, [128, 16, 256])
        rpool = make_rpool(C, es, 'm1_r', 4)
        out_proj_residual(C, es, A, A_t, C.w_out_odd, wpool, rpool)
    P.barrier()


CAP = 640
NJC = CAP // 128
MOE_GROUPS = [(0, 512, 0), (512, CAP - 512, 1)]


def phase_moe(C):
    nc, P = C.nc, C.P
    F = 7168
    xe_scr = nc.dram_tensor("xe_scr", [8, D, CAP], BF16, kind="Internal").ap()
    sg_scr = nc.dram_tensor("sg_scr", [8, CAP, S], BF16, kind="Internal").ap()
    xe_t, sgs_t = Ts(8), Ts(8)
    with ExitStack() as es0:
        hTok = es0.enter_context(SB(nc, 'mo_hTok', [128, 16, 2048], BF16))
        hTok_t = Ts(16)
        L = es0.enter_context(SB(nc, 'mo_L', [128, 16, 8], F32))
        L_t = T()
        with ExitStack() as es1:
            hT = es1.enter_context(SB(nc, 'mo_hT', [128, 16, 2048], BF16))
            hT_t = Ts(4)
            gcol = es1.enter_context(SB(nc, 'mo_g', [128, 16], F32))
            load_cols(C, gcol, C.norm_ffn[1, :])
            gwr = es1.enter_context(SB(nc, 'mo_gwr', [128, 16, 8], F32))
            gwr_t = T()
            P.op('sp', lambda e: e.dma_start(out=gwr[:], in_=C.router_w.rearrange("(c p) e -> p c e", p=128)), [], [gwr_t], dma=True)
            P.barrier()
            for dc in range(16):
                P.op('dve', lambda e: e.tensor_scalar_mul(out=gwr[:, dc, :], in0=gwr[:, dc, :], scalar1=gcol[:, dc:dc + 1]),
                     [gwr_t, C.const_t], [gwr_t])
            LT = es1.enter_context(SB(nc, 'mo_LT', [8, 2048], F32))
            LT_t = T()

            def router(tg, Xb, Xb_t, rb, rb_t):
                ps, ps_t = C.psum('a')
                for dc in range(16):
                    mm(P, ps[0:8, :], gwr[:, dc, :], Xb[:, dc, :], dc == 0, dc == 15, [gwr_t, Xb_t], [ps_t])
                P.op('dve', lambda e: e.tensor_tensor(out=LT[:, tg * 512:(tg + 1) * 512], in0=ps[0:8, :], in1=rb[0:8, :], op=ALU.mult),
                     [ps_t, rb_t], [LT_t])
            make_hT(C, gcol, hT, hT_t, router=router, nbuf=1)
            for t4 in range(4):
                ps, ps_t = C.psum('r')
                for j in range(4):
                    tt = t4 * 4 + j
                    P.op('pe', lambda e: e.transpose(ps[:, j * 8:(j + 1) * 8], LT[0:8, tt * 128:(tt + 1) * 128], C.ident_f[0:8, 0:8]),
                         [LT_t, C.const_t], [ps_t], sig=(j == 3))
                copy_op(P, 'dve', L[:, t4 * 4:(t4 + 1) * 4, :], ps[:, 0:32].rearrange("p (c e) -> p c e", c=4), [ps_t], [L_t])
            k = 0
            for tt in range(16):
                for d4 in range(4):
                    ps, ps_t = C.psum('r')
                    psb = ps[:].bitcast(BF16)
                    for j in range(4):
                        dc = d4 * 4 + j
                        P.op('pe', lambda e: e.transpose(psb[:, j * 128:(j + 1) * 128], hT[:, dc, tt * 128:(tt + 1) * 128], C.ident_b[:]),
                             [hT_t[tt // 4], C.const_t], [ps_t], sig=(j == 3))
                    copy_op(P, evac_engine(k), hTok[:, tt, d4 * 512:(d4 + 1) * 512], psb[:, 0:512], [ps_t], [hTok_t[tt]])
                    k += 1
            P.barrier()
        with ExitStack() as es1:
            def sb(name, shape, dt=F32):
                return es1.enter_context(SB(nc, name, shape, dt))
            m1, m2, dm, ex, g1, g2 = (sb('mo_v%d' % i, [128, 16]) for i in range(6))
            eq1, eq2, L2, Gt, Mt = (sb('mo_w%d' % i, [128, 16, 8]) for i in range(5))
            Mb = sb('mo_Mb', [128, 16, 8], BF16)
            POS = sb('mo_POS', [128, 16, 8])
            UT = sb('mo_UT', [128, 128], BF16)
            iota = sb('mo_iota', [128, CAP])
            P.op('pool', lambda e: e.dma_start(out=UT[:], in_=C.c_ut[:, :]), [], [C.const_t], dma=True)
            P.op('sp', lambda e: e.dma_start(out=iota[:], in_=C.c_iota[:, :]), [], [C.const_t], dma=True)
            R_t = T()

            def dv(fn):
                P.op('dve', fn, [R_t, L_t, C.const_t], [R_t])

            def rmax(dst, src):
                dv(lambda e: e.tensor_tensor(out=dst[:], in0=src[:, :, 0], in1=src[:, :, 1], op=ALU.max))
                for ee in range(2, 8):
                    dv(lambda e: e.tensor_tensor(out=dst[:], in0=dst[:], in1=src[:, :, ee], op=ALU.max))
            rmax(m1, L)
            for ee in range(8):
                dv(lambda e: e.tensor_tensor(out=eq1[:, :, ee], in0=L[:, :, ee], in1=m1[:], op=ALU.is_equal))
            dv(lambda e: e.scalar_tensor_tensor(out=L2[:], in0=eq1[:], scalar=-1e30, in1=L[:], op0=ALU.mult, op1=ALU.add))
            rmax(m2, L2)
            for ee in range(8):
                dv(lambda e: e.tensor_tensor(out=eq2[:, :, ee], in0=L2[:, :, ee], in1=m2[:], op=ALU.is_equal))
            dv(lambda e: e.tensor_tensor(out=dm[:], in0=m2[:], in1=m1[:], op=ALU.subtract))
            P.op('act', lambda e: e.activation(out=ex[:], in_=dm[:], func=AF.Exp), [R_t], [R_t])
            dv(lambda e: e.tensor_scalar_add(out=g1[:], in0=ex[:], scalar1=1.0))
            dv(lambda e: e.reciprocal(out=g1[:], in_=g1[:]))
            dv(lambda e: e.tensor_tensor(out=g2[:], in0=ex[:], in1=g1[:], op=ALU.mult))
            for ee in range(8):
                dv(lambda e: e.tensor_tensor(out=Gt[:, :, ee], in0=eq1[:, :, ee], in1=g1[:], op=ALU.mult))
                dv(lambda e: e.tensor_tensor(out=L2[:, :, ee], in0=eq2[:, :, ee], in1=g2[:], op=ALU.mult))
            dv(lambda e: e.tensor_tensor(out=Gt[:], in0=Gt[:], in1=L2[:], op=ALU.add))
            dv(lambda e: e.tensor_tensor(out=Mt[:], in0=eq1[:], in1=eq2[:], op=ALU.add))
            dv(lambda e: e.tensor_copy(Mb[:], Mt[:]))
            for t4 in range(4):
                ps, ps_t = C.psum('r')
                for j in range(4):
                    tt = t4 * 4 + j
                    for t2 in range(tt):
                        mm(P, ps[:, j * 8:(j + 1) * 8], C.ones_b[:], Mb[:, t2, :], t2 == 0, False, [R_t, C.const_t], [ps_t], sig=False)
                    mm(P, ps[:, j * 8:(j + 1) * 8], UT[:], Mb[:, tt, :], tt == 0, True, [R_t, C.const_t], [ps_t], sig=True)
                P.op('dve', lambda e: e.tensor_copy(POS[:, t4 * 4:(t4 + 1) * 4, :], ps[:, 0:32].rearrange("p (c e) -> p c e", c=4)),
                     [ps_t, R_t], [R_t])
            Sel = [sb('mo_Sel%d' % i, [128, 16, CAP], BF16) for i in range(2)]
            SelG = [sb('mo_SelG%d' % i, [128, 16, CAP], BF16) for i in range(2)]
            XeT = [sb('mo_XeT%d' % i, [128, 16, CAP], BF16) for i in range(1)] * 2
            SGT = [sb('mo_SGT%d' % i, [128, NJC, 2048], BF16) for i in range(1)] * 2
            Sel_t, SelG_t, XeT_t, SGT_t = Ts(2), Ts(2), Ts(1) * 2, Ts(1) * 2
            k = 0
            for ex_ in range(8):
                i2 = ex_ % 2
                for tt in range(16):
                    P.op('dve', lambda e: e.tensor_scalar(out=Sel[i2][:, tt, :], in0=iota[:], scalar1=POS[:, tt, ex_:ex_ + 1],
                                                          scalar2=Mt[:, tt, ex_:ex_ + 1], op0=ALU.is_equal, op1=ALU.mult),
                         [R_t, C.const_t], [Sel_t[i2]])
                    P.op('dve', lambda e: e.tensor_scalar(out=SelG[i2][:, tt, :], in0=iota[:], scalar1=POS[:, tt, ex_:ex_ + 1],
                                                           scalar2=Gt[:, tt, ex_:ex_ + 1], op0=ALU.is_equal, op1=ALU.mult),
                         [R_t, C.const_t], [SelG_t[i2]])
                for dc in range(16):
                    for (c0, n, ti) in MOE_GROUPS:
                        ps, ps_t = C.psum('r')
                        for tt in range(16):
                            mm(P, ps[:, 0:n], hTok[:, tt, dc * 128:(dc + 1) * 128], Sel[i2][:, tt, c0:c0 + n], tt == 0, tt == 15,
                               [hTok_t[tt], Sel_t[i2]], [ps_t])
                        copy_op(P, evac_engine(k), XeT[i2][:, dc, c0:c0 + n], ps[:, 0:n], [ps_t], [XeT_t[i2]])
                        k += 1
                P.op('sp', lambda e: e.dma_start(out=xe_scr[ex_].rearrange("(c p) j -> p c j", p=128), in_=XeT[i2][:]),
                     [XeT_t[i2]], [xe_t[ex_]], dma=True)
                for jc in range(NJC):
                    for t4 in range(4):
                        ps, ps_t = C.psum('a' if k % 2 else 'b')
                        psb = ps[:].bitcast(BF16)
                        for j in range(4):
                            tt = t4 * 4 + j
                            P.op('pe', lambda e: e.transpose(psb[:, j * 128:(j + 1) * 128], SelG[i2][:, tt, jc * 128:(jc + 1) * 128], C.ident_b[:]),
                                 [SelG_t[i2], C.const_t], [ps_t], sig=(j == 3))
                        copy_op(P, evac_engine(k), SGT[i2][:, jc, t4 * 512:(t4 + 1) * 512], psb[:, 0:512], [ps_t], [SGT_t[i2]])
                        k += 1
                P.op('sp', lambda e: e.dma_start(out=sg_scr[ex_].rearrange("(c p) t -> p c t", p=128), in_=SGT[i2][:]),
                     [SGT_t[i2]], [sgs_t[ex_]], dma=True)
            P.barrier()
    with ExitStack() as es0:
        nf = F // 128
        AT = es0.enter_context(SB(nc, 'mo_AT', [128, nf, CAP], BF16))
        Ye = es0.enter_context(SB(nc, 'mo_Ye', [128, NJC, 2048], BF16))
        XS = es0.enter_context(SB(nc, 'mo_XS', [128, 16 * CAP], BF16))
        XeV = XS[:, :].rearrange("p (c j) -> p c j", j=CAP)
        SGV = XS[:, :].rearrange("p (c t) -> p c t", t=2048)
        XS_t = Ts(2)
        AT_t = Ts(2)
        Ye_t = Ts(8)
        up = UPool(C, es0, 'mo_wu', 3, nf * 256)
        rp = ResPipe(C, es0, 'mo_r', 5, [(dchunk, tg) for _ in range(8) for dchunk in range(16) for tg in range(4)])
        sgpool = make_sgpool(C, es0, 'mo_sg', 2, BF16)
        k = 0
        for ex_ in range(8):
            P.op('sp', lambda e: e.dma_start(out=XeV, in_=xe_scr[ex_].rearrange("(c p) j -> p c j", p=128)),
                 [xe_t[ex_]], XS_t, dma=True)
            swiglu_A(C, XeV, XS_t, MOE_GROUPS, C.moe_w_gate[ex_], C.moe_w_up[ex_], F, AT, AT_t, up, sgpool)
            P.op('sp', lambda e: e.dma_start(out=SGV, in_=sg_scr[ex_].rearrange("(c p) t -> p c t", p=128)),
                 [sgs_t[ex_]], XS_t, dma=True)

            def scatter(dt_):
                for m in range(2):
                    dchunk = dt_ * 2 + m
                    for tg in range(4):
                        ps, ps_t = C.psum('c')
                        for jc in range(NJC):
                            mm(P, ps[:], Ye[:, jc, dchunk * 128:(dchunk + 1) * 128], SGV[:, jc, tg * 512:(tg + 1) * 512],
                               jc == 0, jc == NJC - 1, [Ye_t[dt_]] + XS_t, [ps_t])
                        rp.finish(ps, ps_t)
            for dt in range(8):
                wd, wd_ts = up.load_full(C.moe_w_down[ex_][:, dt * 256:(dt + 1) * 256].rearrange("(c p) n -> p c n", p=128), nf)
                for jc in range(NJC):
                    ps, ps_t = C.psum('r')
                    for fc in range(nf):
                        mm(P, ps[:, 0:256], AT[:, fc, jc * 128:(jc + 1) * 128], wd[:, fc, :], fc == 0, fc == nf - 1,
                           wd_ts + [AT_t[0], AT_t[1]], [ps_t])
                    copy_op(P, evac_engine(k), Ye[:, jc, dt * 256:(dt + 1) * 256], ps[:, 0:256], [ps_t], [Ye_t[dt]])
                    k += 1
                if dt >= 1:
                    scatter(dt - 1)
            scatter(7)
        P.barrier()


class UPool:
    def __init__(s, C, es, name, nbuf, nelem):
        s.C = C
        s.bufs = [es.enter_context(SB(C.nc, '%s%d' % (name, i), [128, nelem], BF16)) for i in range(nbuf)]
        s.ts = [Ts(2) for _ in range(nbuf)]
        s.i = 0
        s.half = 0

    def load(s, src_ap):
        i = s.i % len(s.bufs)
        h = s.half
        b = s.bufs[i]
        view = b[:, h * 4096:(h + 1) * 4096].rearrange("p (c n) -> p c n", n=256)
        t = s.ts[i][h]
        s.C.P.op('pool', lambda e: e.dma_start(out=view, in_=src_ap), [], [t], dma=True)
        s.half += 1
        if s.half == 2:
            s.half = 0
            s.i += 1
        return view, t

    def load_full(s, src_ap, nc_):
        assert s.half == 0
        i = s.i % len(s.bufs)
        s.i += 1
        b = s.bufs[i]
        view = b[:, 0:nc_ * 256].rearrange("p (c n) -> p c n", n=256)
        s.C.P.op('pool', lambda e: e.dma_start(out=view, in_=src_ap), [], s.ts[i], dma=True)
        return view, list(s.ts[i])


W_SPECS = [
    ("x", [S, D]),
    ("norm_mix", [2, D]), ("norm_ffn", [2, D]),
    ("w_in_even", [D, 5120]), ("pool_w", [4, 128, 128]), ("pool_scale", [512]),
    ("w_out_even", [D, D]),
    ("ffn_w_gate", [D, 5632]), ("ffn_w_up", [D, 5632]), ("ffn_w_down", [5632, D]),
    ("w_in_odd", [D, 8192]), ("lower_bound_logits", [2, D]), ("hgrn_out_norm", [D]),
    ("w_out_odd", [D, D]), ("router_w", [D, 8]),
    ("moe_w_gate", [8, D, 7168]), ("moe_w_up", [8, D, 7168]), ("moe_w_down", [8, 7168, D]),
    ("norm_final", [D]),
]


PHASE_IN = {
    "in": ["x"], "out": ["norm_final"],
    "mix0": ["norm_mix", "w_in_even", "pool_w", "pool_scale", "w_out_even"],
    "ffn0": ["norm_ffn", "ffn_w_gate", "ffn_w_up", "ffn_w_down"],
    "mix1": ["norm_mix", "w_in_odd", "lower_bound_logits", "hgrn_out_norm", "w_out_odd"],
    "moe": ["norm_ffn", "router_w", "moe_w_gate", "moe_w_up", "moe_w_down"],
}


def phase_inputs(phases):
    need = {"norm_final"}
    for p in phases:
        need.update(PHASE_IN[p])
    return need


def host_consts():
    c = {}
    c["c_ident"] = np.eye(128, dtype=np.float32)
    dl = np.arange(-127, 2176)
    M = ((dl >= 0) & (dl <= 128)).astype(np.float32) + ((dl >= 0) & (dl <= 512) & (dl % 4 == 0)) + ((dl >= 0) & (dl <= 2048) & (dl % 16 == 0))
    kk = np.arange(128)[:, None]
    jj = np.arange(2176)[None, :]
    c["c_MT"] = M[(jj - kk) + 127].astype(np.float32)
    rst = np.ones((128, 2048), np.float32)
    rst[:, ::128] = 0.0
    c["c_rst"] = rst
    ii = np.arange(128)
    c["c_bd"] = (ii[:, None] <= ii[None, :]).astype(np.float32)
    c["c_ut"] = (ii[:, None] < ii[None, :]).astype(np.float32)
    c["c_iota"] = np.ascontiguousarray(np.broadcast_to(np.arange(CAP, dtype=np.float32)[None], (128, CAP)))
    t = np.arange(16)[None, :]
    w = np.array([2, 4, 8, 16])[:, None]
    c["c_rc"] = np.ascontiguousarray(np.broadcast_to((1.0 / np.minimum(t + 1, w))[None], (128, 4, 16))).astype(np.float32)
    return c


def build(phases=("in", "out"), dbg=None):
    nc = bass.Bass("TRN2", target_bir_lowering=False)
    C = Ctx()
    C.nc = nc
    need = phase_inputs(phases)
    for name, shp in W_SPECS:
        if name in need:
            setattr(C, name, nc.dram_tensor(name, shp, F32, kind="ExternalInput").ap())
    hc = host_consts()
    for name, arr in hc.items():
        setattr(C, name, nc.dram_tensor(name, list(arr.shape), F32, kind="ExternalInput").ap())
    C.out = nc.dram_tensor("out", [S, D], F32, kind="ExternalOutput").ap()
    C.out_t = T()
    C.xT = nc.dram_tensor("xT_scr", [D, S], F32, kind="Internal").ap()
    C.xT_t = [Ts(4) for _ in range(16)]
    C.aT_scr = nc.dram_tensor("aT_scr", [D, S], BF16, kind="Internal").ap()
    C.aT_scr_t = Ts(16)
    if dbg:
        C.dbg = nc.dram_tensor("dbg", [D, S], F32, kind="ExternalOutput").ap()
    with ExitStack() as es:
        P = Prog(nc, es)
        C.P = P
        C.ps = [es.enter_context(nc.psum_tensor('ps%d' % i, [128, 512], F32)) for i in range(8)]
        C.ps_t = Ts(8)
        C.ps_pools = {'r': [0, 1, 2, 3], 'a': [4, 5], 'b': [6, 7], 'c': [4, 5, 6, 7], 'all': list(range(8))}
        C.ps_idx = {k: 0 for k in C.ps_pools}

        def psum(pool='r'):
            lst = C.ps_pools[pool]
            i = lst[C.ps_idx[pool] % len(lst)]
            C.ps_idx[pool] += 1
            return C.ps[i], C.ps_t[i]
        C.psum = psum
        C.const_t = T()
        C.ident_f = es.enter_context(SB(nc, 'ident_f', [128, 128], F32))
        C.ident_b = es.enter_context(SB(nc, 'ident_b', [128, 128], BF16))
        C.ones_b = es.enter_context(SB(nc, 'ones_b', [128, 128], BF16))
        C.gfin = es.enter_context(SB(nc, 'gfin', [128, 16], F32))
        P.op('sp', lambda e: e.dma_start(out=C.ident_f[:], in_=C.c_ident[:, :]), [], [C.const_t], dma=True)
        P.op('pool', lambda e: e.dma_start(out=C.ident_b[:], in_=C.c_ident[:, :]), [], [C.const_t], dma=True)
        P.op('sp', lambda e: e.dma_start(out=C.gfin[:], in_=C.norm_final.rearrange("(c p) -> p c", p=128),
                                         allow_slow_non_contiguous=True), [], [C.const_t], dma=True)
        P.op('pool', lambda e: e.memset(C.ones_b[:], 1.0), [], [C.const_t])
        C.eps_col = es.enter_context(SB(nc, 'eps_col', [128, 1], F32))
        P.op('pool', lambda e: e.memset(C.eps_col[:], EPS), [], [C.const_t])
        P.barrier()
        for ph in phases:
            if ph == "in":
                phase_in(C)
            elif ph == "out":
                phase_out(C)
            elif ph == "mix0":
                phase_mix0(C)
            elif ph == "ffn0":
                phase_ffn0(C)
            elif ph == "mix1":
                phase_mix1(C)
            elif ph == "moe":
                phase_moe(C)
        if dbg:
            with SB(nc, 'dbg_b', [128, 16, 512], F32) as db:
                db_t = T()
                dbg_t = T()
                for tg in range(4):
                    P.op('sp', lambda e: e.dma_start(out=db[:], in_=C.xT[:, tg * 512:(tg + 1) * 512].rearrange("(c p) t -> p c t", p=128)),
                         [C.xT_t[dc][tg] for dc in range(16)], [db_t], dma=True)
                    P.op('sp', lambda e: e.dma_start(out=C.dbg[:, tg * 512:(tg + 1) * 512].rearrange("(c p) t -> p c t", p=128), in_=db[:]),
                         [db_t], [dbg_t], dma=True)
        P.barrier()
        print("instr counts", P.nins, "sig counts", P.cnt)
    return nc


_NC_CACHE = {}


def run(inputs, phases, dbg=None, ncores=8, trace=False):
    key = (tuple(phases), dbg)
    if key not in _NC_CACHE:
        _NC_CACHE[key] = build(phases, dbg)
    nc = _NC_CACHE[key]
    hc = host_consts()
    in_maps = []
    for b in range(ncores):
        m = {}
        for name, shp in W_SPECS:
            if name not in phase_inputs(phases):
                continue
            a = inputs[name]
            if name == "x":
                a = a[b]
            elif name in ("norm_mix", "norm_ffn", "lower_bound_logits", "norm_final"):
                a = a
            else:
                a = a[0]
            m[name] = np.ascontiguousarray(a, dtype=np.float32).reshape(shp)
        m.update(hc)
        in_maps.append(m)
    res = run_bass_kernel_spmd(nc, in_maps, core_ids=list(range(ncores)), trace=trace)
    return res


def kernel(**inputs):
    inputs = {k: np.asarray(v) for k, v in inputs.items()}
    res = run(inputs, ("in", "mix0", "ffn0", "mix1", "moe", "out"))
    return np.stack([r["out"] for r in res.results], axis=0)
```

```python
import numpy as np
from contextlib import ExitStack
import concourse.bass as bass
import concourse.mybir as mybir
from concourse.bass_utils import run_bass_kernel_spmd

F32 = mybir.dt.float32
BF16 = mybir.dt.bfloat16
AF = mybir.ActivationFunctionType
ALU = mybir.AluOpType
AX = mybir.AxisListType

S = 2048
D = 2048
NT = 16
EPS = 1e-6


class T:
    __slots__ = ('w', 'r')

    def __init__(s):
        s.w = None
        s.r = {}


def Ts(n):
    return [T() for _ in range(n)]


class Prog:
    ENG = ('pe', 'act', 'dve', 'pool', 'sp')

    def __init__(s, nc, es, ndma=32):
        s.nc = nc
        s.e = {'pe': nc.tensor, 'act': nc.scalar, 'dve': nc.vector, 'pool': nc.gpsimd, 'sp': nc.sync}
        s.sem = {k: es.enter_context(nc.semaphore('s_' + k)) for k in s.ENG}
        s.cnt = {k: 0 for k in s.ENG}
        s.waited = {k: {} for k in s.ENG}
        s.dsem = [es.enter_context(nc.semaphore('d%d' % i)) for i in range(ndma)]
        s.dcnt = [0] * ndma
        s.dnext = 0
        s.nins = {k: 0 for k in s.ENG}

    def _sem(s, k):
        return s.sem[k] if isinstance(k, str) else s.dsem[k]

    def _need(s, eng, waits, t):
        if t is None:
            return
        k, v = t
        if eng == 'pe' and k == 'pe':
            return
        if s.waited[eng].get(k, 0) >= v:
            return
        if waits.get(k, 0) < v:
            waits[k] = v

    def op(s, eng, fn, reads=(), writes=(), sig=True, dma=False):
        waits = {}
        for t in reads:
            s._need(eng, waits, t.w)
        for t in writes:
            s._need(eng, waits, t.w)
            for k, v in t.r.items():
                s._need(eng, waits, (k, v))
        if dma:
            k = s.dnext
            s.dnext = (s.dnext + 1) % len(s.dsem)
            s._need(eng, waits, (k, s.dcnt[k]))
            s.dcnt[k] += 16
            ticket = (k, s.dcnt[k])
        elif sig:
            s.cnt[eng] += 1
            ticket = (eng, s.cnt[eng])
        else:
            ticket = (eng, s.cnt[eng] + 1)
        E = s.e[eng]
        for k, v in waits.items():
            s.waited[eng][k] = v
            E.wait_ge(s._sem(k), v)
        ins = fn(E)
        s.nins[eng] += 1
        if dma:
            ins.then_inc(s.dsem[ticket[0]], 16)
        elif sig:
            ins.then_inc(s.sem[eng], 1)
        for t in reads:
            if t.r.get(ticket[0], 0) < ticket[1]:
                t.r[ticket[0]] = ticket[1]
        for t in writes:
            t.w = ticket
            t.r = {}
        return ticket

    def barrier(s):
        for eng in s.ENG:
            E = s.e[eng]
            for f in s.ENG:
                if f == eng or s.cnt[f] == 0:
                    continue
                if s.waited[eng].get(f, 0) < s.cnt[f]:
                    s.waited[eng][f] = s.cnt[f]
                    E.wait_ge(s.sem[f], s.cnt[f])
            for k in range(len(s.dsem)):
                if s.dcnt[k] and s.waited[eng].get(k, 0) < s.dcnt[k]:
                    s.waited[eng][k] = s.dcnt[k]
                    E.wait_ge(s.dsem[k], s.dcnt[k])


class Ctx:
    pass


_UID = [0]


def SB(nc, name, shape, dt):
    _UID[0] += 1
    return nc.sbuf_tensor('%s_u%d' % (name, _UID[0]), shape, dt)


def mm(P, out, lhsT, rhs, start, stop, reads, writes, sig=None):
    if sig is None:
        sig = stop
    return P.op('pe', lambda e: e.matmul(out, lhsT, rhs, start=start, stop=stop), reads, writes, sig=sig)


def evac_engine(i):
    return 'act' if (i & 1) else 'dve'


def copy_op(P, eng, out, in_, reads, writes):
    if eng == 'act':
        return P.op('act', lambda e: e.copy(out, in_), reads, writes)
    return P.op(eng, lambda e: e.tensor_copy(out, in_), reads, writes)


def phase_in(C):
    nc, P = C.nc, C.P
    with ExitStack() as es:
        xin = [es.enter_context(SB(nc, 'pi_xin%d' % i, [128, D], F32)) for i in range(2)]
        xin_t = Ts(2)
        stg = [es.enter_context(SB(nc, 'pi_stg%d' % i, [128, 16, 512], F32)) for i in range(2)]
        stg_t = Ts(2)
        k = 0
        for tg in range(4):
            sb, sb_t = stg[tg % 2], stg_t[tg % 2]
            for t4 in range(4):
                tt = tg * 4 + t4
                xb, xb_t = xin[tt % 2], xin_t[tt % 2]
                P.op('sp', lambda e: e.dma_start(out=xb[:], in_=C.x[tt * 128:(tt + 1) * 128, :]),
                     [], [xb_t], dma=True)
                for d4 in range(4):
                    ps, ps_t = C.psum()
                    for j in range(4):
                        dc = d4 * 4 + j
                        P.op('pe', lambda e: e.transpose(ps[:, j * 128:(j + 1) * 128],
                                                         xb[:, dc * 128:(dc + 1) * 128], C.ident_f[:]),
                             [xb_t, C.const_t], [ps_t], sig=(j == 3))
                    copy_op(P, evac_engine(k), sb[:, d4 * 4:(d4 + 1) * 4, t4 * 128:(t4 + 1) * 128],
                            ps[:].rearrange("p (c t) -> p c t", c=4), [ps_t], [sb_t])
                    k += 1
            P.op('sp', lambda e: e.dma_start(
                out=C.xT[:, tg * 512:(tg + 1) * 512].rearrange("(c p) t -> p c t", p=128), in_=sb[:]),
                [sb_t], [C.xT_t[dc][tg] for dc in range(16)], dma=True)
    P.barrier()


def load_norm(C, tg, gcol, X, X_t, sq, sq_t, rb, rb_t):
    nc, P = C.nc, C.P
    P.op('sp', lambda e: e.dma_start(out=X[:], in_=C.xT[:, tg * 512:(tg + 1) * 512].rearrange("(c p) t -> p c t", p=128)),
         [C.xT_t[dc][tg] for dc in range(16)], [X_t], dma=True)
    for h in range(2):
        P.op('act', lambda e: e.activation(out=sq[:, h * 8:(h + 1) * 8, :], in_=X[:, h * 8:(h + 1) * 8, :], func=AF.Square),
             [X_t], [sq_t[h]])
    ps, ps_t = C.psum()
    for dc in range(16):
        mm(P, ps[:], C.ones_b[:], sq[:, dc, :], dc == 0, dc == 15, [sq_t[dc // 8], C.const_t], [ps_t])
    P.op('act', lambda e: e.activation(out=rb[:], in_=ps[:], func=AF.Ln, scale=1.0 / D, bias=C.eps_col[:, 0:1]),
         [ps_t, C.const_t], [rb_t])
    P.op('act', lambda e: e.activation(out=rb[:], in_=rb[:], func=AF.Exp, scale=-0.5), [rb_t], [rb_t])


def phase_out(C):
    nc, P = C.nc, C.P
    with ExitStack() as es:
        X = [es.enter_context(SB(nc, 'po_X%d' % i, [128, 16, 512], F32)) for i in range(2)]
        X_t = Ts(2)
        sq = es.enter_context(SB(nc, 'po_sq', [128, 16, 512], BF16))
        sq_t = Ts(2)
        rb = es.enter_context(SB(nc, 'po_rb', [128, 512], F32))
        rb_t = T()
        og = [es.enter_context(SB(nc, 'po_o%d' % i, [128, D], F32)) for i in range(2)]
        og_t = Ts(2)
        k = 0
        for tg in range(4):
            Xb, Xb_t = X[tg % 2], X_t[tg % 2]
            load_norm(C, tg, None, Xb, Xb_t, sq, sq_t, rb, rb_t)
            for dc in range(16):
                P.op('dve', lambda e: e.scalar_tensor_tensor(out=Xb[:, dc, :], in0=Xb[:, dc, :], scalar=C.gfin[:, dc:dc + 1],
                                                           in1=rb[:], op0=ALU.mult, op1=ALU.mult),
                     [Xb_t, rb_t, C.const_t], [Xb_t])
            for t4 in range(4):
                tt = tg * 4 + t4
                ob, ob_t = og[tt % 2], og_t[tt % 2]
                for d4 in range(4):
                    ps, ps_t = C.psum()
                    for j in range(4):
                        dc = d4 * 4 + j
                        P.op('pe', lambda e: e.transpose(ps[:, j * 128:(j + 1) * 128],
                                                         Xb[:, dc, t4 * 128:(t4 + 1) * 128], C.ident_f[:]),
                             [Xb_t, C.const_t], [ps_t], sig=(j == 3))
                    copy_op(P, evac_engine(k), ob[:, d4 * 512:(d4 + 1) * 512], ps[:], [ps_t], [ob_t])
                    k += 1
                P.op('sp', lambda e: e.dma_start(out=C.out[tt * 128:(tt + 1) * 128, :], in_=ob[:]),
                     [ob_t], [C.out_t], dma=True)
    P.barrier()


class WPool:
    def __init__(s, C, es, name, nbuf, shape):
        s.C = C
        s.bufs = [es.enter_context(SB(C.nc, '%s%d' % (name, i), shape, BF16)) for i in range(nbuf)]
        s.ts = Ts(nbuf)
        s.i = 0

    def load(s, src_ap, view=None):
        i = s.i % len(s.bufs)
        s.i += 1
        b, t = s.bufs[i], s.ts[i]
        dst = b[:] if view is None else view(b)
        s.C.P.op('pool', lambda e: e.dma_start(out=dst, in_=src_ap), [], [t], dma=True)
        return b, t


def make_hT(C, g_col, hT, hT_t, tgs=(0, 1, 2, 3), router=None, nbuf=2):
    nc, P = C.nc, C.P
    with ExitStack() as es:
        X = [es.enter_context(SB(nc, 'nh_X%d' % i, [128, 16, 512], F32)) for i in range(nbuf)]
        X_t = Ts(nbuf)
        sq = es.enter_context(SB(nc, 'nh_sq', [128, 16, 512], BF16))
        sq_t = Ts(2)
        rb = [es.enter_context(SB(nc, 'nh_rb%d' % i, [128, 512], F32)) for i in range(2)]
        rb_t = Ts(2)
        for i, tg in enumerate(tgs):
            Xb, Xb_t = X[i % nbuf], X_t[i % nbuf]
            load_norm(C, tg, None, Xb, Xb_t, sq, sq_t, rb[i % 2], rb_t[i % 2])
            if router is not None:
                router(tg, Xb, Xb_t, rb[i % 2], rb_t[i % 2])
            for dc in range(16):
                P.op('dve', lambda e: e.scalar_tensor_tensor(out=hT[:, dc, i * 512:(i + 1) * 512], in0=Xb[:, dc, :],
                                                           scalar=g_col[:, dc:dc + 1], in1=rb[i % 2][:],
                                                           op0=ALU.mult, op1=ALU.mult),
                     [Xb_t, rb_t[i % 2], C.const_t], [hT_t[i]])
    P.barrier()


def residual_out(C, dchunk, tg, ps, ps_t, rpool):
    P = C.P
    b, b_t = rpool()
    reg = C.xT_t[dchunk][tg]
    ap = C.xT[dchunk * 128:(dchunk + 1) * 128, tg * 512:(tg + 1) * 512]
    P.op('sp', lambda e: e.dma_start(out=b[:], in_=ap), [reg], [b_t], dma=True)
    P.op('dve', lambda e: e.tensor_tensor(out=b[:], in0=ps[:], in1=b[:], op=ALU.add), [ps_t, b_t], [b_t])
    P.op('sp', lambda e: e.dma_start(out=ap, in_=b[:]), [b_t], [reg], dma=True)


class ResPipe:
    def __init__(s, C, es, name, n, order):
        s.C = C
        s.n = n
        s.bufs = [es.enter_context(SB(C.nc, '%s%d' % (name, i), [128, 512], F32)) for i in range(n)]
        s.ts = Ts(n)
        s.order = list(order)
        s.issued = 0
        s.done = 0

    def _ap(s, i):
        dchunk, tg = s.order[i]
        return s.C.xT[dchunk * 128:(dchunk + 1) * 128, tg * 512:(tg + 1) * 512], s.C.xT_t[dchunk][tg]

    def _issue(s):
        i = s.issued
        b, b_t = s.bufs[i % s.n], s.ts[i % s.n]
        ap, reg = s._ap(i)
        s.C.P.op('sp', lambda e: e.dma_start(out=b[:], in_=ap), [reg], [b_t], dma=True)
        s.issued += 1

    def finish(s, ps, ps_t):
        i = s.done
        while s.issued < min(len(s.order), i + s.n - 1) or s.issued <= i:
            s._issue()
        b, b_t = s.bufs[i % s.n], s.ts[i % s.n]
        ap, reg = s._ap(i)
        P = s.C.P
        P.op('dve', lambda e: e.tensor_tensor(out=b[:], in0=ps[:], in1=b[:], op=ALU.add), [ps_t, b_t], [b_t])
        P.op('sp', lambda e: e.dma_start(out=ap, in_=b[:]), [b_t], [reg], dma=True)
        s.done += 1


def make_rpool(C, es, name, n=4):
    bufs = [es.enter_context(SB(C.nc, '%s%d' % (name, i), [128, 512], F32)) for i in range(n)]
    ts = Ts(n)
    st = [0]

    def get():
        i = st[0] % n
        st[0] += 1
        return bufs[i], ts[i]
    return get


def out_proj_residual(C, es, A, A_t, W, wpool, rpool):
    P = C.P
    rp = ResPipe(C, es, 'opr', 12, [(wt * 2 + m, tg) for wt in range(8) for m in range(2) for tg in range(4)])
    for wt in range(8):
        wb, wb_t = wpool.load(W[:, wt * 256:(wt + 1) * 256].rearrange("(c p) n -> p c n", p=128))
        for m in range(2):
            for tg in range(4):
                ps, ps_t = C.psum('r')
                for fc in range(16):
                    mm(P, ps[:], wb[:, fc, m * 128:(m + 1) * 128], A[:, fc, tg * 512:(tg + 1) * 512],
                       fc == 0, fc == 15, [wb_t, A_t[tg]], [ps_t])
                rp.finish(ps, ps_t)


def phase_mix0(C):
    nc, P = C.nc, C.P
    aT = C.aT_scr
    aT_t = C.aT_scr_t
    with ExitStack() as es0:
        hT = es0.enter_context(SB(nc, 'm0_hT', [128, 16, 2048], BF16))
        hT_t = Ts(4)
        gcol = es0.enter_context(SB(nc, 'm0_g', [128, 16], F32))
        P.op('sp', lambda e: e.dma_start(out=gcol[:], in_=C.norm_mix[0, :].rearrange("(c p) -> p c", p=128),
                                         allow_slow_non_contiguous=True), [], [C.const_t], dma=True)
        make_hT(C, gcol, hT, hT_t)
        with ExitStack() as es:
            wpool = WPool(C, es, 'm0_w', 5, [128, 16, 256])
            QT = [es.enter_context(SB(nc, 'm0_QT', [128, 2, 2048], BF16)) for _ in range(2)]
            KT = [es.enter_context(SB(nc, 'm0_KT', [128, 2, 2048], BF16)) for _ in range(2)]
            V = [es.enter_context(SB(nc, 'm0_V', [128, 16, 256], BF16)) for _ in range(2)]
            QT_t, KT_t, V_t = [Ts(2), Ts(2)], [Ts(2), Ts(2)], Ts(2)
            MT = es.enter_context(SB(nc, 'm0_MT', [128, 2176], BF16))
            P.op('pool', lambda e: e.dma_start(out=MT[:], in_=C.c_MT[:, :]), [], [C.const_t], dma=True)
            NE = 6
            Eb = [es.enter_context(SB(nc, 'm0_E%d' % i, [128, 512], BF16)) for i in range(NE)]
            Eb_t = Ts(NE)
            rc = es.enter_context(SB(nc, 'm0_rc', [128, 512], F32))
            rc_t = T()
            ah = [es.enter_context(SB(nc, 'm0_ah%d' % i, [128, 2048], BF16)) for i in range(2)]
            ah_t = Ts(2)
            st = {'ecnt': 0, 'hcnt': 0, 'k': 0}

            def proj_steps(g):
                gb = g % 2
                wq, wq_t = wpool.load(C.w_in_even[:, g * 256:(g + 1) * 256].rearrange("(c p) n -> p c n", p=128))
                wk, wk_t = wpool.load(C.w_in_even[:, 1536 + g * 256:1536 + (g + 1) * 256].rearrange("(c p) n -> p c n", p=128))
                wv, wv_t = wpool.load(C.w_in_even[:, 3072 + g * 256:3072 + (g + 1) * 256].rearrange("(c p) n -> p c n", p=128))
                for (wb, wb_t, dst, dst_t, scale) in ((wq, wq_t, QT[gb], QT_t[gb], 128.0 ** -0.5), (wk, wk_t, KT[gb], KT_t[gb], 1.0)):
                    for hh in range(2):
                        for tg in range(4):
                            ps, ps_t = C.psum('r')
                            for dc in range(16):
                                mm(P, ps[:], wb[:, dc, hh * 128:(hh + 1) * 128], hT[:, dc, tg * 512:(tg + 1) * 512],
                                   dc == 0, dc == 15, [wb_t, hT_t[tg]], [ps_t])
                            if st['k'] % 2 == 0:
                                P.op('act', lambda e: e.activation(out=dst[:, hh, tg * 512:(tg + 1) * 512], in_=ps[:],
                                                                   func=AF.Copy, scale=scale), [ps_t], [dst_t[hh]])
                            else:
                                P.op('dve', lambda e: e.tensor_scalar_mul(out=dst[:, hh, tg * 512:(tg + 1) * 512], in0=ps[:],
                                                                          scalar1=scale), [ps_t], [dst_t[hh]])
                            st['k'] += 1
                            yield
                for tt in range(16):
                    ps, ps_t = C.psum('r')
                    for dc in range(16):
                        mm(P, ps[:, 0:256], hT[:, dc, tt * 128:(tt + 1) * 128], wv[:, dc, :], dc == 0, dc == 15,
                           [wv_t, hT_t[tt // 4]], [ps_t])
                    copy_op(P, evac_engine(tt), V[gb][:, tt, :], ps[:, 0:256], [ps_t], [V_t[gb]])
                    yield

            def attn_steps(g):
                gb = g % 2
                for hh in range(2):
                    head = g * 2 + hh
                    ab, ab_t = ah[st['hcnt'] % 2], ah_t[st['hcnt'] % 2]
                    st['hcnt'] += 1
                    for qg in range(4):
                        psO, psO_t = C.psum('a')
                        psD, psD_t = C.psum('b')
                        nk = 4 * qg + 4
                        pend = None
                        for kt in range(nk + 1):
                            cur = None
                            if kt < nk:
                                q0 = max(512 * qg, 128 * kt)
                                N = 512 * qg + 512 - q0
                                ps, ps_t = C.psum('r')
                                mm(P, ps[:, 0:N], KT[gb][:, hh, kt * 128:(kt + 1) * 128], QT[gb][:, hh, q0:q0 + N], True, True,
                                   [KT_t[gb][hh], QT_t[gb][hh]], [ps_t])
                                E, E_t = Eb[st['ecnt'] % NE], Eb_t[st['ecnt'] % NE]
                                st['ecnt'] += 1
                                P.op('act', lambda e: e.activation(out=E[:, 0:N], in_=ps[:, 0:N], func=AF.Exp), [ps_t], [E_t])
                                d0 = q0 - 128 * kt
                                P.op('dve', lambda e: e.tensor_tensor(out=E[:, 0:N], in0=E[:, 0:N], in1=MT[:, d0:d0 + N], op=ALU.mult),
                                     [E_t, C.const_t], [E_t])
                                cur = (kt, q0, N, E, E_t)
                            if pend is not None:
                                pk, pq0, pN, pE, pE_t = pend
                                c0 = pq0 - 512 * qg
                                mm(P, psO[:, c0:c0 + pN], V[gb][:, pk, hh * 128:(hh + 1) * 128], pE[:, 0:pN], pk == 0, pk == nk - 1,
                                   [V_t[gb], pE_t], [psO_t])
                                mm(P, psD[:, c0:c0 + pN], C.ones_b[:], pE[:, 0:pN], pk == 0, pk == nk - 1,
                                   [C.const_t, pE_t], [psD_t])
                            pend = cur
                            yield
                        P.op('dve', lambda e: e.reciprocal(out=rc[:], in_=psD[:]), [psD_t], [rc_t])
                        P.op('dve', lambda e: e.tensor_tensor(out=ab[:, qg * 512:(qg + 1) * 512], in0=psO[:], in1=rc[:], op=ALU.mult),
                             [psO_t, rc_t], [ab_t])
                    P.op('sp', lambda e: e.dma_start(out=aT[head * 128:(head + 1) * 128, :], in_=ab[:]), [ab_t], [aT_t[head]], dma=True)

            pw = es.enter_context(SB(nc, 'm0_pw', [128, 4, 128], BF16))
            psc = es.enter_context(SB(nc, 'm0_psc', [128, 4], F32))
            rct = es.enter_context(SB(nc, 'm0_rct', [128, 4, 16], F32))
            P.op('pool', lambda e: e.dma_start(out=pw[:], in_=C.pool_w.rearrange("g i o -> i g o")), [], [C.const_t], dma=True)
            P.op('sp', lambda e: e.dma_start(out=psc[:], in_=C.pool_scale.rearrange("(g p) -> p g", p=128),
                                             allow_slow_non_contiguous=True), [], [C.const_t], dma=True)
            P.op('sp', lambda e: e.dma_start(out=rct[:], in_=C.c_rc[:, :, :]), [], [C.const_t], dma=True)
            pb = [es.enter_context(SB(nc, 'm0_pb%d' % i, [128, 16 + 2048], F32)) for i in range(3)]
            pb_t = Ts(3)
            for i in range(3):
                P.op('pool', lambda e: e.memset(pb[i][:, 0:16], 0.0), [], [pb_t[i]])
            t16 = es.enter_context(SB(nc, 'm0_t16', [128, 16], F32))
            t16_t = T()
            pa = [es.enter_context(SB(nc, 'm0_pa%d' % i, [128, 2048], BF16)) for i in range(2)]
            pa_t = Ts(2)

            def pool_steps():
                wp = wp_t = None
                for gi in range(4):
                    if gi % 2 == 0:
                        wp, wp_t = wpool.load(C.w_in_even[:, 4608 + (gi // 2) * 256:4608 + (gi // 2 + 1) * 256].rearrange("(c p) n -> p c n", p=128))
                    w = 2 << gi
                    p0, p0_t = pb[0], pb_t[0]
                    for tg in range(4):
                        ps, ps_t = C.psum('r')
                        for dc in range(16):
                            mm(P, ps[:], wp[:, dc, (gi % 2) * 128:(gi % 2 + 1) * 128], hT[:, dc, tg * 512:(tg + 1) * 512],
                               dc == 0, dc == 15, [wp_t, hT_t[tg]], [ps_t])
                        copy_op(P, evac_engine(tg), p0[:, 16 + tg * 512:16 + (tg + 1) * 512], ps[:], [ps_t], [p0_t])
                        yield
                    src, src_t = p0, p0_t
                    step = 1
                    alt = 1
                    while step < w:
                        dst, dst_t = pb[alt], pb_t[alt]
                        P.op('dve', lambda e: e.tensor_tensor(out=dst[:, 16:16 + 2048], in0=src[:, 16:16 + 2048],
                                                              in1=src[:, 16 - step:16 - step + 2048], op=ALU.add),
                             [src_t], [dst_t])
                        src, src_t = dst, dst_t
                        alt = 3 - alt
                        step *= 2
                        yield
                    ab, ab_t = pa[0], pa_t[0]
                    P.op('dve', lambda e: e.scalar_tensor_tensor(out=ab[:], in0=src[:, 16:16 + 2048], scalar=1.0 / w,
                                                               in1=p0[:, 16:16 + 2048], op0=ALU.mult, op1=ALU.subtract),
                         [src_t, p0_t], [ab_t])
                    yield
                    P.op('dve', lambda e: e.tensor_tensor(out=t16[:], in0=src[:, 16:32], in1=rct[:, gi, :], op=ALU.mult),
                         [src_t, C.const_t], [t16_t])
                    P.op('dve', lambda e: e.tensor_tensor(out=ab[:, 0:16], in0=t16[:], in1=p0[:, 16:32], op=ALU.subtract),
                         [t16_t, p0_t], [ab_t])
                    yield
                    ob, ob_t = pa[1], pa_t[1]
                    for tg in range(4):
                        ps, ps_t = C.psum('r')
                        mm(P, ps[:], pw[:, gi, :], ab[:, tg * 512:(tg + 1) * 512], True, True, [C.const_t, ab_t], [ps_t])
                        P.op('act', lambda e: e.activation(out=ob[:, tg * 512:(tg + 1) * 512], in_=ps[:], func=AF.Copy,
                                                           scale=psc[:, gi:gi + 1]), [ps_t, C.const_t], [ob_t])
                        yield
                    P.op('sp', lambda e: e.dma_start(out=aT[(12 + gi) * 128:(13 + gi) * 128, :], in_=ob[:]), [ob_t], [aT_t[12 + gi]], dma=True)
                    yield

            for _ in proj_steps(0):
                pass
            for g in range(6):
                pg = proj_steps(g + 1) if g + 1 < 6 else pool_steps()
                every = 3 if g + 1 < 6 else 1
                i = 0
                for _ in attn_steps(g):
                    i += 1
                    if pg is not None and i % every == 0:
                        if next(pg, 'end') == 'end':
                            pg = None
                if pg is not None:
                    for _ in pg:
                        pass
        P.barrier()
    with ExitStack() as es:
        A = es.enter_context(SB(nc, 'm0_A', [128, 16, 2048], BF16))
        A_t = Ts(4)
        for fc in range(16):
            P.op('sp', lambda e: e.dma_start(out=A[:, fc, :], in_=aT[fc * 128:(fc + 1) * 128, :]), [aT_t[fc]], A_t, dma=True)
        wpool = WPool(C, es, 'm0_wo', 3, [128, 16, 256])
        rpool = make_rpool(C, es, 'm0_r', 4)
        out_proj_residual(C, es, A, A_t, C.w_out_even, wpool, rpool)
    P.barrier()


def swiglu_A(C, XT, XT_t, groups, Wg, Wu, F, AT, AT_t, wpool, sgpool):
    P = C.P
    k = 0
    for ft in range(F // 256):
        wg, wg_t = wpool.load(Wg[:, ft * 256:(ft + 1) * 256].rearrange("(c p) n -> p c n", p=128))
        wu, wu_t = wpool.load(Wu[:, ft * 256:(ft + 1) * 256].rearrange("(c p) n -> p c n", p=128))
        for m in range(2):
            ffc = ft * 2 + m
            for (c0, n, ti) in groups:
                psG, psG_t = C.psum('r')
                for dc in range(16):
                    mm(P, psG[:, 0:n], wg[:, dc, m * 128:(m + 1) * 128], XT[:, dc, c0:c0 + n], dc == 0, dc == 15,
                       [wg_t, XT_t[ti]], [psG_t])
                psU, psU_t = C.psum('a' if k % 2 == 0 else 'b')
                for dc in range(16):
                    mm(P, psU[:, 0:n], wu[:, dc, m * 128:(m + 1) * 128], XT[:, dc, c0:c0 + n], dc == 0, dc == 15,
                       [wu_t, XT_t[ti]], [psU_t])
                sg, sg_t = sgpool()
                P.op('act', lambda e: e.activation(out=sg[:, 0:n], in_=psG[:, 0:n], func=AF.Silu), [psG_t], [sg_t])
                P.op('dve', lambda e: e.tensor_tensor(out=AT[:, ffc, c0:c0 + n], in0=psU[:, 0:n], in1=sg[:, 0:n], op=ALU.mult),
                     [psU_t, sg_t], [AT_t[ti]])
                k += 1


def down_fm(C, AT, AT_t, groups, Wd, F, wpool, out_fn):
    P = C.P
    nf = F // 128
    for dt in range(8):
        wd, wd_t = wpool.load(Wd[:, dt * 256:(dt + 1) * 256].rearrange("(c p) n -> p c n", p=128))
        for m in range(2):
            for gi, (c0, n, ti) in enumerate(groups):
                ps, ps_t = C.psum('r')
                for fc in range(nf):
                    mm(P, ps[:, 0:n], wd[:, fc, m * 128:(m + 1) * 128], AT[:, fc, c0:c0 + n], fc == 0, fc == nf - 1,
                       [wd_t, AT_t[ti]], [ps_t])
                out_fn(dt * 2 + m, gi, ps, ps_t)


def make_sgpool(C, es, name, n=3, dt=F32):
    bufs = [es.enter_context(SB(C.nc, '%s%d' % (name, i), [128, 512], dt)) for i in range(n)]
    ts = Ts(n)
    st = [0]

    def get():
        i = st[0] % n
        st[0] += 1
        return bufs[i], ts[i]
    return get


def phase_ffn0(C):
    nc, P = C.nc, C.P
    F = 5632
    with ExitStack() as es0:
        gcol = es0.enter_context(SB(nc, 'f0_g', [128, 16], F32))
        P.op('sp', lambda e: e.dma_start(out=gcol[:], in_=C.norm_ffn[0, :].rearrange("(c p) -> p c", p=128),
                                         allow_slow_non_contiguous=True), [], [C.const_t], dma=True)
        hT = es0.enter_context(SB(nc, 'f0_hT', [128, 16, 1024], BF16))
        AT = es0.enter_context(SB(nc, 'f0_AT', [128, F // 128, 1024], BF16))
        for job in range(2):
            hT_t = Ts(2)
            AT_t = Ts(2)
            make_hT(C, gcol, hT, hT_t, tgs=(2 * job, 2 * job + 1))
            groups = [(0, 512, 0), (512, 512, 1)]
            with ExitStack() as es:
                wpool = WPool(C, es, 'f0_w', 4, [128, 16, 256])
                sgpool = make_sgpool(C, es, 'f0_sg', 3)
                swiglu_A(C, hT, hT_t, groups, C.ffn_w_gate, C.ffn_w_up, F, AT, AT_t, wpool, sgpool)
                P.barrier()
            with ExitStack() as es:
                wpool = WPool(C, es, 'f0_wd', 2, [128, F // 128, 256])
                rp = ResPipe(C, es, 'f0_r', 8, [(dt * 2 + m, 2 * job + gi) for dt in range(8) for m in range(2) for gi in range(2)])
                down_fm(C, AT, AT_t, groups, C.ffn_w_down, F, wpool,
                        lambda dchunk, gi, ps, ps_t: rp.finish(ps, ps_t))
                P.barrier()


def load_cols(C, dst, src_1d):
    C.P.op('sp', lambda e: e.dma_start(out=dst[:], in_=src_1d.rearrange("(c p) -> p c", p=128),
                                       allow_slow_non_contiguous=True), [], [C.const_t], dma=True)


def phase_mix1(C):
    nc, P = C.nc, C.P
    aT = C.aT_scr
    aT_t = C.aT_scr_t
    with ExitStack() as es0:
        hT = es0.enter_context(SB(nc, 'm1_hT', [128, 16, 2048], BF16))
        hT_t = Ts(4)
        gcol = es0.enter_context(SB(nc, 'm1_g', [128, 16], F32))
        ocol = es0.enter_context(SB(nc, 'm1_oc', [128, 16], F32))
        lb = es0.enter_context(SB(nc, 'm1_lb', [128, 16], F32))
        oml = es0.enter_context(SB(nc, 'm1_oml', [128, 16], F32))
        l0 = es0.enter_context(SB(nc, 'm1_l0', [128, 16], F32))
        load_cols(C, gcol, C.norm_mix[1, :])
        load_cols(C, ocol, C.hgrn_out_norm)
        load_cols(C, l0, C.lower_bound_logits[0, :])
        load_cols(C, lb, C.lower_bound_logits[1, :])
        P.barrier()
        P.op('dve', lambda e: e.tensor_tensor(out=lb[:], in0=lb[:], in1=l0[:], op=ALU.subtract), [C.const_t], [C.const_t])
        P.op('act', lambda e: e.activation(out=lb[:], in_=lb[:], func=AF.Sigmoid), [C.const_t], [C.const_t])
        P.op('dve', lambda e: e.tensor_scalar(out=oml[:], in0=lb[:], scalar1=-1.0, scalar2=1.0, op0=ALU.mult, op1=ALU.add),
             [C.const_t], [C.const_t])
        P.barrier()
        make_hT(C, gcol, hT, hT_t)
        with ExitStack() as es:
            wpool = WPool(C, es, 'm1_w', 4, [128, 16, 256])
            V = es.enter_context(SB(nc, 'm1_V', [128, 16, 256], BF16))
            V_t = T()
            B = [es.enter_context(SB(nc, 'm1_B%d' % i, [128, 2048], F32)) for i in range(4)]
            B_t = Ts(4)
            rst = es.enter_context(SB(nc, 'm1_rst', [128, 2048], F32))
            bd = es.enter_context(SB(nc, 'm1_bd', [128, 128], BF16))
            P.op('sp', lambda e: e.dma_start(out=rst[:], in_=C.c_rst[:, :]), [], [C.const_t], dma=True)
            P.op('pool', lambda e: e.dma_start(out=bd[:], in_=C.c_bd[:, :]), [], [C.const_t], dma=True)
            class HB:
                pass
            H = []
            for i in range(2):
                hb = HB()
                hb.KbT = es.enter_context(SB(nc, 'm1_KbT', [128, 2048], BF16))
                hb.QhT = es.enter_context(SB(nc, 'm1_QhT', [128, 2048], BF16))
                hb.KbTok = es.enter_context(SB(nc, 'm1_KbTok', [128, 16, 128], BF16))
                hb.SG = es.enter_context(SB(nc, 'm1_SG', [128, 2048], BF16))
                hb.Osb = es.enter_context(SB(nc, 'm1_O', [128, 2048], F32))
                hb.dn = es.enter_context(SB(nc, 'm1_dn', [128, 32], F32))
                hb.Sst = es.enter_context(SB(nc, 'm1_S', [128, 128], F32))
                hb.Sp = es.enter_context(SB(nc, 'm1_Sp', [128, 128], F32))
                hb.Sbf = es.enter_context(SB(nc, 'm1_Sbf', [128, 128], BF16))
                hb.attm = [es.enter_context(SB(nc, 'm1_att%d' % j, [128, 128], BF16)) for j in range(2)]
                hb.attm_t = Ts(2)
                (hb.KbT_t, hb.QhT_t, hb.KbTok_t, hb.SG_t, hb.Osb_t, hb.dn_t, hb.S_t, hb.Sp_t, hb.Sbf_t) = (T() for _ in range(9))
                H.append(hb)
            osq = es.enter_context(SB(nc, 'm1_osq', [128, 2048], BF16))
            orb = es.enter_context(SB(nc, 'm1_orb', [128, 512], F32))
            of = [es.enter_context(SB(nc, 'm1_of%d' % i, [128, 2048], BF16)) for i in range(1)] * 2
            of_t = Ts(1) * 2
            osq_t, orb_t = T(), T()
            eps128 = C.eps_col
            for g in range(8):
                wq, wq_t = wpool.load(C.w_in_odd[:, g * 256:(g + 1) * 256].rearrange("(c p) n -> p c n", p=128))
                wf, wf_t = wpool.load(C.w_in_odd[:, 2048 + g * 256:2048 + (g + 1) * 256].rearrange("(c p) n -> p c n", p=128))
                wi, wi_t = wpool.load(C.w_in_odd[:, 4096 + g * 256:4096 + (g + 1) * 256].rearrange("(c p) n -> p c n", p=128))
                wg, wg_t = wpool.load(C.w_in_odd[:, 6144 + g * 256:6144 + (g + 1) * 256].rearrange("(c p) n -> p c n", p=128))
                for tt in range(16):
                    ps, ps_t = C.psum('all')
                    for dc in range(16):
                        mm(P, ps[:, 0:256], hT[:, dc, tt * 128:(tt + 1) * 128], wi[:, dc, :], dc == 0, dc == 15,
                           [wi_t, hT_t[tt // 4]], [ps_t])
                    copy_op(P, evac_engine(tt), V[:, tt, :], ps[:, 0:256], [ps_t], [V_t])
                for hh in range(2):
                    h = 2 * g + hh
                    hb = H[hh]
                    hs = slice(hh * 128, (hh + 1) * 128)

                    def proj(wb, wb_t, func, dst, dst_t):
                        for tg in range(4):
                            ps, ps_t = C.psum('all')
                            for dc in range(16):
                                mm(P, ps[:], wb[:, dc, hs], hT[:, dc, tg * 512:(tg + 1) * 512], dc == 0, dc == 15,
                                   [wb_t, hT_t[tg]], [ps_t])
                            P.op('act', lambda e: e.activation(out=dst[:, tg * 512:(tg + 1) * 512], in_=ps[:], func=func),
                                 [ps_t], [dst_t])
                    proj(wf, wf_t, AF.Sigmoid, B[0], B_t[0])
                    P.op('dve', lambda e: e.tensor_scalar(out=B[0][:], in0=B[0][:], scalar1=oml[:, h:h + 1], scalar2=lb[:, h:h + 1],
                                                          op0=ALU.mult, op1=ALU.add), [B_t[0], C.const_t], [B_t[0]])
                    P.op('act', lambda e: e.activation(out=B[1][:], in_=B[0][:], func=AF.Ln), [B_t[0]], [B_t[1]])
                    P.op('dve', lambda e: e.tensor_tensor_scan(out=B[2][:], data0=rst[:], data1=B[1][:], initial=0.0,
                                                              op0=ALU.mult, op1=ALU.add), [B_t[1], C.const_t], [B_t[2]])
                    P.op('act', lambda e: e.activation(out=B[1][:], in_=B[2][:], func=AF.Exp), [B_t[2]], [B_t[1]])
                    P.op('act', lambda e: e.activation(out=B[3][:], in_=B[2][:], func=AF.Exp, scale=-1.0), [B_t[2]], [B_t[3]])
                    P.op('dve', lambda e: e.tensor_scalar(out=B[0][:], in0=B[0][:], scalar1=-1.0, scalar2=1.0, op0=ALU.mult, op1=ALU.add),
                         [B_t[0]], [B_t[0]])
                    P.op('dve', lambda e: e.tensor_tensor(out=hb.KbT[:], in0=B[0][:], in1=B[3][:], op=ALU.mult), [B_t[0], B_t[3]], [hb.KbT_t])
                    P.op('dve', lambda e: e.tensor_copy(hb.dn[:, 0:16], B[1][:].rearrange("p (n c) -> p n c", c=128)[:, :, 127]), [B_t[1]], [hb.dn_t])
                    proj(wq, wq_t, AF.Silu, B[2], B_t[2])
                    P.op('dve', lambda e: e.tensor_tensor(out=hb.QhT[:], in0=B[2][:], in1=B[1][:], op=ALU.mult), [B_t[2], B_t[1]], [hb.QhT_t])
                    proj(wg, wg_t, AF.Silu, hb.SG, hb.SG_t)
                    for t4 in range(4):
                        ps, ps_t = C.psum('all')
                        psb = ps[:].bitcast(BF16)
                        for j in range(4):
                            tt = t4 * 4 + j
                            P.op('pe', lambda e: e.transpose(psb[:, j * 128:(j + 1) * 128], hb.KbT[:, tt * 128:(tt + 1) * 128], C.ident_b[:]),
                                 [hb.KbT_t, C.const_t], [ps_t], sig=(j == 3))
                        copy_op(P, evac_engine(t4), hb.KbTok[:, t4 * 4:(t4 + 1) * 4, :], psb[:, 0:512].rearrange("p (c t) -> p c t", c=4),
                                [ps_t], [hb.KbTok_t])
                    P.op('pool', lambda e: e.memset(hb.Sst[:], 0.0), [], [hb.S_t])
                    P.op('pool', lambda e: e.memset(hb.Sbf[:], 0.0), [], [hb.Sbf_t])
                for m in range(16):
                    ms = slice(m * 128, (m + 1) * 128)
                    for hh in range(2):
                        hb = H[hh]
                        hs = slice(hh * 128, (hh + 1) * 128)
                        psA, psA_t = C.psum('r')
                        mm(P, psA[:, 0:128], hb.KbT[:, ms], hb.QhT[:, ms], True, True, [hb.KbT_t, hb.QhT_t], [psA_t])
                        am, am_t = hb.attm[m % 2], hb.attm_t[m % 2]
                        P.op('dve', lambda e: e.tensor_tensor(out=am[:], in0=psA[:, 0:128], in1=bd[:], op=ALU.mult),
                             [psA_t, C.const_t], [am_t])
                        psO, psO_t = C.psum('a')
                        mm(P, psO[:, 0:128], V[:, m, hs], am[:], True, False, [V_t, am_t], [psO_t], sig=False)
                        mm(P, psO[:, 0:128], hb.Sbf[:], hb.QhT[:, ms], False, True, [hb.Sbf_t, hb.QhT_t], [psO_t], sig=True)
                        if m < 15:
                            psU, psU_t = C.psum('b')
                            mm(P, psU[:, 0:128], hb.KbTok[:, m, :], V[:, m, hs], True, True, [hb.KbTok_t, V_t], [psU_t])
                            P.op('dve', lambda e: e.tensor_tensor(out=hb.Sp[:], in0=psU[:, 0:128], in1=hb.Sst[:], op=ALU.add),
                                 [psU_t, hb.S_t], [hb.Sp_t])
                            P.op('act', lambda e: e.activation(out=hb.Sbf[:], in_=hb.Sp[:], func=AF.Copy, scale=hb.dn[:, m:m + 1]),
                                 [hb.Sp_t, hb.dn_t], [hb.Sbf_t])
                            P.op('dve', lambda e: e.tensor_scalar_mul(out=hb.Sst[:], in0=hb.Sp[:], scalar1=hb.dn[:, m:m + 1]),
                                 [hb.Sp_t, hb.dn_t], [hb.S_t])
                        copy_op(P, 'act', hb.Osb[:, ms], psO[:, 0:128], [psO_t], [hb.Osb_t])
                for hh in range(2):
                    h = 2 * g + hh
                    hb = H[hh]
                    P.op('act', lambda e: e.activation(out=osq[:], in_=hb.Osb[:], func=AF.Square), [hb.Osb_t], [osq_t])
                    ob, ob_t = of[h % 2], of_t[h % 2]
                    for tg in range(4):
                        ts_ = slice(tg * 512, (tg + 1) * 512)
                        ps, ps_t = C.psum('r')
                        mm(P, ps[:], C.ones_b[:], osq[:, ts_], True, True, [C.const_t, osq_t], [ps_t])
                        P.op('act', lambda e: e.activation(out=orb[:], in_=ps[:], func=AF.Ln, scale=1.0 / 128, bias=eps128[:, 0:1]),
                             [ps_t, C.const_t], [orb_t])
                        P.op('act', lambda e: e.activation(out=orb[:], in_=orb[:], func=AF.Exp, scale=-0.5), [orb_t], [orb_t])
                        P.op('dve', lambda e: e.scalar_tensor_tensor(out=hb.Osb[:, ts_], in0=hb.Osb[:, ts_], scalar=ocol[:, h:h + 1], in1=orb[:],
                                                                   op0=ALU.mult, op1=ALU.mult), [hb.Osb_t, orb_t, C.const_t], [hb.Osb_t])
                        P.op('dve', lambda e: e.tensor_tensor(out=ob[:, ts_], in0=hb.Osb[:, ts_], in1=hb.SG[:, ts_], op=ALU.mult),
                             [hb.Osb_t, hb.SG_t], [ob_t])
                    P.op('sp', lambda e: e.dma_start(out=aT[h * 128:(h + 1) * 128, :], in_=ob[:]), [ob_t], [aT_t[h]], dma=True)
        P.barrier()
    with ExitStack() as es:
        A = es.enter_context(SB(nc, 'm1_A', [128, 16, 2048], BF16))
        A_t = Ts(4)
        for fc in range(16):
            P.op('sp', lambda e: e.dma_start(out=A[:, fc, :], in_=aT[fc * 128:(fc + 1) * 128, :]), [aT_t[fc]], A_t, dma=True)
        wpool = WPool(C, es, 'm1_wo', 3, [128, 16, 256])
        rpool = make_rpool(C, es, 'm1_r', 4)
        out_proj_residual(C, es, A, A_t, C.w_out_odd, wpool, rpool)
    P.barrier()


CAP = 640
NJC = CAP // 128
MOE_GROUPS = [(0, 512, 0), (512, CAP - 512, 1)]


def phase_moe(C):
    nc, P = C.nc, C.P
    F = 7168
    xe_scr = nc.dram_tensor("xe_scr", [8, D, CAP], BF16, kind="Internal").ap()
    sg_scr = nc.dram_tensor("sg_scr", [8, CAP, S], BF16, kind="Internal").ap()
    xe_t, sgs_t = Ts(8), Ts(8)
    with ExitStack() as es0:
        hTok = es0.enter_context(SB(nc, 'mo_hTok', [128, 16, 2048], BF16))
        hTok_t = Ts(16)
        L = es0.enter_context(SB(nc, 'mo_L', [128, 16, 8], F32))
        L_t = T()
        with ExitStack() as es1:
            hT = es1.enter_context(SB(nc, 'mo_hT', [128, 16, 2048], BF16))
            hT_t = Ts(4)
            gcol = es1.enter_context(SB(nc, 'mo_g', [128, 16], F32))
            load_cols(C, gcol, C.norm_ffn[1, :])
            gwr = es1.enter_context(SB(nc, 'mo_gwr', [128, 16, 8], F32))
            gwr_t = T()
            P.op('sp', lambda e: e.dma_start(out=gwr[:], in_=C.router_w.rearrange("(c p) e -> p c e", p=128)), [], [gwr_t], dma=True)
            P.barrier()
            for dc in range(16):
                P.op('dve', lambda e: e.tensor_scalar_mul(out=gwr[:, dc, :], in0=gwr[:, dc, :], scalar1=gcol[:, dc:dc + 1]),
                     [gwr_t, C.const_t], [gwr_t])
            LT = es1.enter_context(SB(nc, 'mo_LT', [8, 2048], F32))
            LT_t = T()

            def router(tg, Xb, Xb_t, rb, rb_t):
                ps, ps_t = C.psum('a')
                for dc in range(16):
                    mm(P, ps[0:8, :], gwr[:, dc, :], Xb[:, dc, :], dc == 0, dc == 15, [gwr_t, Xb_t], [ps_t])
                P.op('dve', lambda e: e.tensor_tensor(out=LT[:, tg * 512:(tg + 1) * 512], in0=ps[0:8, :], in1=rb[0:8, :], op=ALU.mult),
                     [ps_t, rb_t], [LT_t])
            make_hT(C, gcol, hT, hT_t, router=router, nbuf=1)
            for t4 in range(4):
                ps, ps_t = C.psum('r')
                for j in range(4):
                    tt = t4 * 4 + j
                    P.op('pe', lambda e: e.transpose(ps[:, j * 8:(j + 1) * 8], LT[0:8, tt * 128:(tt + 1) * 128], C.ident_f[0:8, 0:8]),
                         [LT_t, C.const_t], [ps_t], sig=(j == 3))
                copy_op(P, 'dve', L[:, t4 * 4:(t4 + 1) * 4, :], ps[:, 0:32].rearrange("p (c e) -> p c e", c=4), [ps_t], [L_t])
            k = 0
            for tt in range(16):
                for d4 in range(4):
                    ps, ps_t = C.psum('r')
                    psb = ps[:].bitcast(BF16)
                    for j in range(4):
                        dc = d4 * 4 + j
                        P.op('pe', lambda e: e.transpose(psb[:, j * 128:(j + 1) * 128], hT[:, dc, tt * 128:(tt + 1) * 128], C.ident_b[:]),
                             [hT_t[tt // 4], C.const_t], [ps_t], sig=(j == 3))
                    copy_op(P, evac_engine(k), hTok[:, tt, d4 * 512:(d4 + 1) * 512], psb[:, 0:512], [ps_t], [hTok_t[tt]])
                    k += 1
            P.barrier()
        with ExitStack() as es1:
            def sb(name, shape, dt=F32):
                return es1.enter_context(SB(nc, name, shape, dt))
            m1, m2, dm, ex, g1, g2 = (sb('mo_v%d' % i, [128, 16]) for i in range(6))
            eq1, eq2, L2, Gt, Mt = (sb('mo_w%d' % i, [128, 16, 8]) for i in range(5))
            Mb = sb('mo_Mb', [128, 16, 8], BF16)
            POS = sb('mo_POS', [128, 16, 8])
            UT = sb('mo_UT', [128, 128], BF16)
            iota = sb('mo_iota', [128, CAP])
            P.op('pool', lambda e: e.dma_start(out=UT[:], in_=C.c_ut[:, :]), [], [C.const_t], dma=True)
            P.op('sp', lambda e: e.dma_start(out=iota[:], in_=C.c_iota[:, :]), [], [C.const_t], dma=True)
            R_t = T()

            def dv(fn):
                P.op('dve', fn, [R_t, L_t, C.const_t], [R_t])

            def rmax(dst, src):
                dv(lambda e: e.tensor_tensor(out=dst[:], in0=src[:, :, 0], in1=src[:, :, 1], op=ALU.max))
                for ee in range(2, 8):
                    dv(lambda e: e.tensor_tensor(out=dst[:], in0=dst[:], in1=src[:, :, ee], op=ALU.max))
            rmax(m1, L)
            for ee in range(8):
                dv(lambda e: e.tensor_tensor(out=eq1[:, :, ee], in0=L[:, :, ee], in1=m1[:], op=ALU.is_equal))
            dv(lambda e: e.scalar_tensor_tensor(out=L2[:], in0=eq1[:], scalar=-1e30, in1=L[:], op0=ALU.mult, op1=ALU.add))
            rmax(m2, L2)
            for ee in range(8):
                dv(lambda e: e.tensor_tensor(out=eq2[:, :, ee], in0=L2[:, :, ee], in1=m2[:], op=ALU.is_equal))
            dv(lambda e: e.tensor_tensor(out=dm[:], in0=m2[:], in1=m1[:], op=ALU.subtract))
            P.op('act', lambda e: e.activation(out=ex[:], in_=dm[:], func=AF.Exp), [R_t], [R_t])
            dv(lambda e: e.tensor_scalar_add(out=g1[:], in0=ex[:], scalar1=1.0))
            dv(lambda e: e.reciprocal(out=g1[:], in_=g1[:]))
            dv(lambda e: e.tensor_tensor(out=g2[:], in0=ex[:], in1=g1[:], op=ALU.mult))
            for ee in range(8):
                dv(lambda e: e.tensor_tensor(out=Gt[:, :, ee], in0=eq1[:, :, ee], in1=g1[:], op=ALU.mult))
                dv(lambda e: e.tensor_tensor(out=L2[:, :, ee], in0=eq2[:, :, ee], in1=g2[:], op=ALU.mult))
            dv(lambda e: e.tensor_tensor(out=Gt[:], in0=Gt[:], in1=L2[:], op=ALU.add))
            dv(lambda e: e.tensor_tensor(out=Mt[:], in0=eq1[:], in1=eq2[:], op=ALU.add))
            dv(lambda e: e.tensor_copy(Mb[:], Mt[:]))
            for t4 in range(4):
                ps, ps_t = C.psum('r')
                for j in range(4):
                    tt = t4 * 4 + j
                    for t2 in range(tt):
                        mm(P, ps[:, j * 8:(j + 1) * 8], C.ones_b[:], Mb[:, t2, :], t2 == 0, False, [R_t, C.const_t], [ps_t], sig=False)
                    mm(P, ps[:, j * 8:(j + 1) * 8], UT[:], Mb[:, tt, :], tt == 0, True, [R_t, C.const_t], [ps_t], sig=True)
                P.op('dve', lambda e: e.tensor_copy(POS[:, t4 * 4:(t4 + 1) * 4, :], ps[:, 0:32].rearrange("p (c e) -> p c e", c=4)),
                     [ps_t, R_t], [R_t])
            Sel = [sb('mo_Sel%d' % i, [128, 16, CAP], BF16) for i in range(2)]
            SelG = [sb('mo_SelG%d' % i, [128, 16, CAP], BF16) for i in range(2)]
            XeT = [sb('mo_XeT%d' % i, [128, 16, CAP], BF16) for i in range(1)] * 2
            SGT = [sb('mo_SGT%d' % i, [128, NJC, 2048], BF16) for i in range(1)] * 2
            Sel_t, SelG_t, XeT_t, SGT_t = Ts(2), Ts(2), Ts(1) * 2, Ts(1) * 2
            k = 0
            for ex_ in range(8):
                i2 = ex_ % 2
                for tt in range(16):
                    P.op('dve', lambda e: e.tensor_scalar(out=Sel[i2][:, tt, :], in0=iota[:], scalar1=POS[:, tt, ex_:ex_ + 1],
                                                          scalar2=Mt[:, tt, ex_:ex_ + 1], op0=ALU.is_equal, op1=ALU.mult),
                         [R_t, C.const_t], [Sel_t[i2]])
                    P.op('dve', lambda e: e.tensor_scalar(out=SelG[i2][:, tt, :], in0=iota[:], scalar1=POS[:, tt, ex_:ex_ + 1],
                                                           scalar2=Gt[:, tt, ex_:ex_ + 1], op0=ALU.is_equal, op1=ALU.mult),
                         [R_t, C.const_t], [SelG_t[i2]])
                for dc in range(16):
                    for (c0, n, ti) in MOE_GROUPS:
                        ps, ps_t = C.psum('r')
                        t0 = c0 // 128
                        for tt in range(t0, 16):
                            mm(P, ps[:, 0:n], hTok[:, tt, dc * 128:(dc + 1) * 128], Sel[i2][:, tt, c0:c0 + n], tt == t0, tt == 15,
                               [hTok_t[tt], Sel_t[i2]], [ps_t])
                        copy_op(P, evac_engine(k), XeT[i2][:, dc, c0:c0 + n], ps[:, 0:n], [ps_t], [XeT_t[i2]])
                        k += 1
                P.op('sp', lambda e: e.dma_start(out=xe_scr[ex_].rearrange("(c p) j -> p c j", p=128), in_=XeT[i2][:]),
                     [XeT_t[i2]], [xe_t[ex_]], dma=True)
                for jc in range(NJC):
                    for t4 in range(4):
                        ps, ps_t = C.psum('a' if k % 2 else 'b')
                        psb = ps[:].bitcast(BF16)
                        for j in range(4):
                            tt = t4 * 4 + j
                            P.op('pe', lambda e: e.transpose(psb[:, j * 128:(j + 1) * 128], SelG[i2][:, tt, jc * 128:(jc + 1) * 128], C.ident_b[:]),
                                 [SelG_t[i2], C.const_t], [ps_t], sig=(j == 3))
                        copy_op(P, evac_engine(k), SGT[i2][:, jc, t4 * 512:(t4 + 1) * 512], psb[:, 0:512], [ps_t], [SGT_t[i2]])
                        k += 1
                P.op('sp', lambda e: e.dma_start(out=sg_scr[ex_].rearrange("(c p) t -> p c t", p=128), in_=SGT[i2][:]),
                     [SGT_t[i2]], [sgs_t[ex_]], dma=True)
            P.barrier()
    with ExitStack() as es0:
        nf = F // 128
        AT = es0.enter_context(SB(nc, 'mo_AT', [128, nf, CAP], BF16))
        Ye = es0.enter_context(SB(nc, 'mo_Ye', [128, NJC, 2048], BF16))
        XS = es0.enter_context(SB(nc, 'mo_XS', [128, 16 * CAP], BF16))
        XeV = XS[:, :].rearrange("p (c j) -> p c j", j=CAP)
        SGV = XS[:, :].rearrange("p (c t) -> p c t", t=2048)
        XS_t = Ts(2)
        AT_t = Ts(2)
        Ye_t = Ts(8)
        up = UPool(C, es0, 'mo_wu', 3, nf * 256)
        rp = ResPipe(C, es0, 'mo_r', 5, [(dchunk, tg) for _ in range(8) for dchunk in range(16) for tg in range(4)])
        sgpool = make_sgpool(C, es0, 'mo_sg', 2, BF16)
        k = 0
        for ex_ in range(8):
            P.op('sp', lambda e: e.dma_start(out=XeV, in_=xe_scr[ex_].rearrange("(c p) j -> p c j", p=128)),
                 [xe_t[ex_]], XS_t, dma=True)
            swiglu_A(C, XeV, XS_t, MOE_GROUPS, C.moe_w_gate[ex_], C.moe_w_up[ex_], F, AT, AT_t, up, sgpool)
            P.op('sp', lambda e: e.dma_start(out=SGV, in_=sg_scr[ex_].rearrange("(c p) t -> p c t", p=128)),
                 [sgs_t[ex_]], XS_t, dma=True)

            def scatter(dt_):
                for m in range(2):
                    dchunk = dt_ * 2 + m
                    for tg in range(4):
                        ps, ps_t = C.psum('c')
                        nj = min(NJC, 4 * (tg + 1))
                        for jc in range(nj):
                            mm(P, ps[:], Ye[:, jc, dchunk * 128:(dchunk + 1) * 128], SGV[:, jc, tg * 512:(tg + 1) * 512],
                               jc == 0, jc == nj - 1, [Ye_t[dt_]] + XS_t, [ps_t])
                        rp.finish(ps, ps_t)
            for dt in range(8):
                wd, wd_ts = up.load_full(C.moe_w_down[ex_][:, dt * 256:(dt + 1) * 256].rearrange("(c p) n -> p c n", p=128), nf)
                for jc in range(NJC):
                    ps, ps_t = C.psum('r')
                    for fc in range(nf):
                        mm(P, ps[:, 0:256], AT[:, fc, jc * 128:(jc + 1) * 128], wd[:, fc, :], fc == 0, fc == nf - 1,
                           wd_ts + [AT_t[0], AT_t[1]], [ps_t])
                    copy_op(P, evac_engine(k), Ye[:, jc, dt * 256:(dt + 1) * 256], ps[:, 0:256], [ps_t], [Ye_t[dt]])
                    k += 1
                if dt >= 1:
                    scatter(dt - 1)
            scatter(7)
        P.barrier()


class UPool:
    def __init__(s, C, es, name, nbuf, nelem):
        s.C = C
        s.bufs = [es.enter_context(SB(C.nc, '%s%d' % (name, i), [128, nelem], BF16)) for i in range(nbuf)]
        s.ts = [Ts(2) for _ in range(nbuf)]
        s.i = 0
        s.half = 0

    def load(s, src_ap):
        i = s.i % len(s.bufs)
        h = s.half
        b = s.bufs[i]
        view = b[:, h * 4096:(h + 1) * 4096].rearrange("p (c n) -> p c n", n=256)
        t = s.ts[i][h]
        s.C.P.op('pool', lambda e: e.dma_start(out=view, in_=src_ap), [], [t], dma=True)
        s.half += 1
        if s.half == 2:
            s.half = 0
            s.i += 1
        return view, t

    def load_full(s, src_ap, nc_):
        assert s.half == 0
        i = s.i % len(s.bufs)
        s.i += 1
        b = s.bufs[i]
        view = b[:, 0:nc_ * 256].rearrange("p (c n) -> p c n", n=256)
        s.C.P.op('pool', lambda e: e.dma_start(out=view, in_=src_ap), [], s.ts[i], dma=True)
        return view, list(s.ts[i])


W_SPECS = [
    ("x", [S, D]),
    ("norm_mix", [2, D]), ("norm_ffn", [2, D]),
    ("w_in_even", [D, 5120]), ("pool_w", [4, 128, 128]), ("pool_scale", [512]),
    ("w_out_even", [D, D]),
    ("ffn_w_gate", [D, 5632]), ("ffn_w_up", [D, 5632]), ("ffn_w_down", [5632, D]),
    ("w_in_odd", [D, 8192]), ("lower_bound_logits", [2, D]), ("hgrn_out_norm", [D]),
    ("w_out_odd", [D, D]), ("router_w", [D, 8]),
    ("moe_w_gate", [8, D, 7168]), ("moe_w_up", [8, D, 7168]), ("moe_w_down", [8, 7168, D]),
    ("norm_final", [D]),
]


PHASE_IN = {
    "in": ["x"], "out": ["norm_final"],
    "mix0": ["norm_mix", "w_in_even", "pool_w", "pool_scale", "w_out_even"],
    "ffn0": ["norm_ffn", "ffn_w_gate", "ffn_w_up", "ffn_w_down"],
    "mix1": ["norm_mix", "w_in_odd", "lower_bound_logits", "hgrn_out_norm", "w_out_odd"],
    "moe": ["norm_ffn", "router_w", "moe_w_gate", "moe_w_up", "moe_w_down"],
}


def phase_inputs(phases):
    need = {"norm_final"}
    for p in phases:
        need.update(PHASE_IN[p])
    return need


def host_consts():
    c = {}
    c["c_ident"] = np.eye(128, dtype=np.float32)
    dl = np.arange(-127, 2176)
    M = ((dl >= 0) & (dl <= 128)).astype(np.float32) + ((dl >= 0) & (dl <= 512) & (dl % 4 == 0)) + ((dl >= 0) & (dl <= 2048) & (dl % 16 == 0))
    kk = np.arange(128)[:, None]
    jj = np.arange(2176)[None, :]
    c["c_MT"] = M[(jj - kk) + 127].astype(np.float32)
    rst = np.ones((128, 2048), np.float32)
    rst[:, ::128] = 0.0
    c["c_rst"] = rst
    ii = np.arange(128)
    c["c_bd"] = (ii[:, None] <= ii[None, :]).astype(np.float32)
    c["c_ut"] = (ii[:, None] < ii[None, :]).astype(np.float32)
    c["c_iota"] = np.ascontiguousarray(np.broadcast_to(np.arange(CAP, dtype=np.float32)[None], (128, CAP)))
    t = np.arange(16)[None, :]
    w = np.array([2, 4, 8, 16])[:, None]
    c["c_rc"] = np.ascontiguousarray(np.broadcast_to((1.0 / np.minimum(t + 1, w))[None], (128, 4, 16))).astype(np.float32)
    return c


def build(phases=("in", "out"), dbg=None):
    nc = bass.Bass("TRN2", target_bir_lowering=False)
    C = Ctx()
    C.nc = nc
    need = phase_inputs(phases)
    for name, shp in W_SPECS:
        if name in need:
            setattr(C, name, nc.dram_tensor(name, shp, F32, kind="ExternalInput").ap())
    hc = host_consts()
    for name, arr in hc.items():
        setattr(C, name, nc.dram_tensor(name, list(arr.shape), F32, kind="ExternalInput").ap())
    C.out = nc.dram_tensor("out", [S, D], F32, kind="ExternalOutput").ap()
    C.out_t = T()
    C.xT = nc.dram_tensor("xT_scr", [D, S], F32, kind="Internal").ap()
    C.xT_t = [Ts(4) for _ in range(16)]
    C.aT_scr = nc.dram_tensor("aT_scr", [D, S], BF16, kind="Internal").ap()
    C.aT_scr_t = Ts(16)
    if dbg:
        C.dbg = nc.dram_tensor("dbg", [D, S], F32, kind="ExternalOutput").ap()
    with ExitStack() as es:
        P = Prog(nc, es)
        C.P = P
        C.ps = [es.enter_context(nc.psum_tensor('ps%d' % i, [128, 512], F32)) for i in range(8)]
        C.ps_t = Ts(8)
        C.ps_pools = {'r': [0, 1, 2, 3], 'a': [4, 5], 'b': [6, 7], 'c': [4, 5, 6, 7], 'all': list(range(8))}
        C.ps_idx = {k: 0 for k in C.ps_pools}

        def psum(pool='r'):
            lst = C.ps_pools[pool]
            i = lst[C.ps_idx[pool] % len(lst)]
            C.ps_idx[pool] += 1
            return C.ps[i], C.ps_t[i]
        C.psum = psum
        C.const_t = T()
        C.ident_f = es.enter_context(SB(nc, 'ident_f', [128, 128], F32))
        C.ident_b = es.enter_context(SB(nc, 'ident_b', [128, 128], BF16))
        C.ones_b = es.enter_context(SB(nc, 'ones_b', [128, 128], BF16))
        C.gfin = es.enter_context(SB(nc, 'gfin', [128, 16], F32))
        P.op('sp', lambda e: e.dma_start(out=C.ident_f[:], in_=C.c_ident[:, :]), [], [C.const_t], dma=True)
        P.op('pool', lambda e: e.dma_start(out=C.ident_b[:], in_=C.c_ident[:, :]), [], [C.const_t], dma=True)
        P.op('sp', lambda e: e.dma_start(out=C.gfin[:], in_=C.norm_final.rearrange("(c p) -> p c", p=128),
                                         allow_slow_non_contiguous=True), [], [C.const_t], dma=True)
        P.op('pool', lambda e: e.memset(C.ones_b[:], 1.0), [], [C.const_t])
        C.eps_col = es.enter_context(SB(nc, 'eps_col', [128, 1], F32))
        P.op('pool', lambda e: e.memset(C.eps_col[:], EPS), [], [C.const_t])
        P.barrier()
        for ph in phases:
            if ph == "in":
                phase_in(C)
            elif ph == "out":
                phase_out(C)
            elif ph == "mix0":
                phase_mix0(C)
            elif ph == "ffn0":
                phase_ffn0(C)
            elif ph == "mix1":
                phase_mix1(C)
            elif ph == "moe":
                phase_moe(C)
        if dbg:
            with SB(nc, 'dbg_b', [128, 16, 512], F32) as db:
                db_t = T()
                dbg_t = T()
                for tg in range(4):
                    P.op('sp', lambda e: e.dma_start(out=db[:], in_=C.xT[:, tg * 512:(tg + 1) * 512].rearrange("(c p) t -> p c t", p=128)),
                         [C.xT_t[dc][tg] for dc in range(16)], [db_t], dma=True)
                    P.op('sp', lambda e: e.dma_start(out=C.dbg[:, tg * 512:(tg + 1) * 512].rearrange("(c p) t -> p c t", p=128), in_=db[:]),
                         [db_t], [dbg_t], dma=True)
        P.barrier()
        print("instr counts", P.nins, "sig counts", P.cnt)
    return nc


_NC_CACHE = {}


def run(inputs, phases, dbg=None, ncores=8, trace=False):
    key = (tuple(phases), dbg)
    if key not in _NC_CACHE:
        _NC_CACHE[key] = build(phases, dbg)
    nc = _NC_CACHE[key]
    hc = host_consts()
    in_maps = []
    for b in range(ncores):
        m = {}
        for name, shp in W_SPECS:
            if name not in phase_inputs(phases):
                continue
            a = inputs[name]
            if name == "x":
                a = a[b]
            elif name in ("norm_mix", "norm_ffn", "lower_bound_logits", "norm_final"):
                a = a
            else:
                a = a[0]
            m[name] = np.ascontiguousarray(a, dtype=np.float32).reshape(shp)
        m.update(hc)
        in_maps.append(m)
    res = run_bass_kernel_spmd(nc, in_maps, core_ids=list(range(ncores)), trace=trace)
    return res


def kernel(**inputs):
    inputs = {k: np.asarray(v) for k, v in inputs.items()}
    res = run(inputs, ("in", "mix0", "ffn0", "mix1", "moe", "out"))
    return np.stack([r["out"] for r in res.results], axis=0)
```
